# Optimizing a Trainium2 kernel written in Bass

```python
import math
import jax, jax.numpy as jnp
from jax import lax
import numpy as np

D_MODEL = 1024
BATCH = 8
SEQ = 8192
DEPTH = 1

MIX_WIDTH = D_MODEL
SGU_GROUPS = 4
SGU_CH = D_MODEL // 8
SGU_WIDTH = SGU_GROUPS * SGU_CH
CHUNK = 128
DIFF_HEADS = 4
DIFF_QK_DIM = D_MODEL // 16
DIFF_V_DIM = 2 * DIFF_QK_DIM
DIFF_QK_WIDTH = DIFF_HEADS * 2 * DIFF_QK_DIM
DIFF_WIDTH = DIFF_HEADS * DIFF_V_DIM
Q_BLOCK = 128
IN_WIDTH = 2 * SGU_WIDTH + 2 * DIFF_QK_WIDTH + DIFF_WIDTH
MEM_LEN = 256
MEM_HEADS = 4
MEM_HEAD_DIM = D_MODEL // MEM_HEADS
MEM_WIDTH = MEM_HEADS * MEM_HEAD_DIM
N_GROUPS = 4
EXPERTS_PER_GROUP = 8
N_EXPERTS = N_GROUPS * EXPERTS_PER_GROUP
TOP_K = 2
D_EXPERT = D_MODEL // 2
EXPERT_BLOCK = 128
RMS_EPS = 1e-6
LN_EPS = 1e-5

kernel_name = "hymba_sgu_diffattn_alibi_memxattn_hmoe"


def rms_norm(x, g, eps=RMS_EPS):
    xf = x.astype(jnp.float32)
    y = xf * lax.rsqrt(jnp.mean(xf * xf, axis=-1, keepdims=True) + eps)
    return (y * g.astype(jnp.float32)).astype(x.dtype)


def layer_norm(x, g, b, eps=LN_EPS):
    xf = x.astype(jnp.float32)
    mu = jnp.mean(xf, axis=-1, keepdims=True)
    var = jnp.mean(jnp.square(xf - mu), axis=-1, keepdims=True)
    y = (xf - mu) * lax.rsqrt(var + eps)
    return (y * g.astype(jnp.float32) + b.astype(jnp.float32)).astype(x.dtype)


def chunked_sgu(u, v, ln_g, ln_b, w_s, b_s):
    Bn, Sn, G, C = v.shape
    vn = layer_norm(v, ln_g, ln_b)
    vc = vn.reshape(Bn, Sn // CHUNK, CHUNK, G, C)
    ws = jnp.tril(w_s).astype(vc.dtype)
    sp = jnp.einsum('gts,bnsgc->bntgc', ws, vc) + b_s.T.astype(vc.dtype)[:, :, None]
    return u * sp.reshape(Bn, Sn, G, C)


def diff_attention(q, k, v, lam):
    Bn, H, _, Sn, Dq = q.shape
    n_qb = Sn // Q_BLOCK
    slopes = 2.0 ** (-8.0 * jnp.arange(1, H + 1, dtype=jnp.float32) / H)
    q_blocks = jnp.moveaxis(q.reshape(Bn, H, 2, n_qb, Q_BLOCK, Dq), 3, 0)
    kpos = jnp.arange(Sn, dtype=jnp.int32)

    def attend(args):
        qb, i = args
        s = jnp.einsum('bhiqd,bhikd->bhiqk', qb, k).astype(jnp.float32)
        qpos = i * Q_BLOCK + jnp.arange(Q_BLOCK, dtype=jnp.int32)
        dist = (qpos[:, None] - kpos[None, :]).astype(jnp.float32)
        s = s - (slopes[:, None, None] * dist)[None, :, None]
        s = jnp.where(dist >= 0, s, -jnp.inf)
        p = jax.nn.softmax(s, axis=-1)
        a = p[:, :, 0] - lam * p[:, :, 1]
        return jnp.einsum('bhqk,bhkd->bqhd', a.astype(v.dtype), v)

    o = lax.map(attend, (q_blocks, jnp.arange(n_qb, dtype=jnp.int32)))
    return jnp.moveaxis(o, 0, 1).reshape(Bn, Sn, H, v.shape[-1])


def memory_attention(hq, mem_n, w_q, w_kv, w_o):
    Bn, Sn, _ = hq.shape
    Mn = mem_n.shape[1]
    q = (hq @ w_q).reshape(Bn, Sn, MEM_HEADS, MEM_HEAD_DIM) * (MEM_HEAD_DIM ** -0.5)
    kv = (mem_n @ w_kv).reshape(Bn, Mn, 2, MEM_HEADS, MEM_HEAD_DIM)
    k, v = kv[:, :, 0], kv[:, :, 1]
    s = jnp.einsum('bqhd,bkhd->bhqk', q, k).astype(jnp.float32)
    p = jax.nn.softmax(s, axis=-1)
    o = jnp.einsum('bhqk,bkhd->bqhd', p.astype(v.dtype), v).reshape(Bn, Sn, MEM_WIDTH)
    return o @ w_o


def hierarchical_moe(h, w_rg, b_rg, w_re, b_re, w_gate, w_up, w_down):
    Bn, Sn, D = h.shape
    t = h.reshape(-1, D)
    N = t.shape[0]
    pg = jax.nn.softmax((t @ w_rg + b_rg).astype(jnp.float32), axis=-1)
    gate_g, gidx = lax.top_k(pg, 1)
    el = (t @ w_re + b_re).astype(jnp.float32).reshape(N, N_GROUPS, EXPERTS_PER_GROUP)
    el = el[jnp.arange(N), gidx[:, 0]]
    pe = jax.nn.softmax(el, axis=-1)
    pe_top, eloc = lax.top_k(pe, TOP_K)
    wts = gate_g * pe_top / jnp.sum(pe_top, axis=-1, keepdims=True)
    eid = (gidx * EXPERTS_PER_GROUP + eloc).reshape(-1)
    A = N * TOP_K
    tok = jnp.repeat(jnp.arange(N, dtype=jnp.int32), TOP_K)
    order = jnp.argsort(eid)
    e_sorted = eid[order]
    counts = jnp.bincount(eid, length=N_EXPERTS)
    starts = jnp.cumsum(counts) - counts
    padded = (counts + EXPERT_BLOCK - 1) // EXPERT_BLOCK * EXPERT_BLOCK
    pend = jnp.cumsum(padded)
    pstarts = pend - padded
    pos_sorted = (pstarts[e_sorted] + jnp.arange(A, dtype=jnp.int32) - starts[e_sorted]).astype(jnp.int32)
    P = -(-A // EXPERT_BLOCK) * EXPERT_BLOCK + N_EXPERTS * EXPERT_BLOCK
    n_blocks = P // EXPERT_BLOCK
    row_tok = jnp.zeros((P,), jnp.int32).at[pos_sorted].set(tok[order])
    block_expert = jnp.clip(jnp.searchsorted(pend, jnp.arange(n_blocks, dtype=jnp.int32) * EXPERT_BLOCK,
                                             side='right'), 0, N_EXPERTS - 1).astype(jnp.int32)
    x_rows = t[row_tok].reshape(n_blocks, EXPERT_BLOCK, D)

    def expert_rows(args):
        xb, e = args
        g = xb @ w_gate[e]
        u = xb @ w_up[e]
        return (jax.nn.silu(g) * u) @ w_down[e]

    y_rows = lax.map(expert_rows, (x_rows, block_expert)).reshape(P, D)
    pos = jnp.zeros((A,), jnp.int32).at[order].set(pos_sorted)
    y = y_rows[pos].reshape(N, TOP_K, D)
    out = jnp.einsum('nk,nkd->nd', wts.astype(y.dtype), y)
    return out.reshape(Bn, Sn, D)


def setup_inputs(seed: int = 0) -> dict:
    key = jax.random.key(seed)
    ks = jax.random.split(key, 32)
    L, D = DEPTH, D_MODEL
    f32 = jnp.float32

    def nrm(k, shape, scale):
        return jax.random.normal(k, shape, f32) * scale

    def gain(k, shape):
        return 1.0 + 0.05 * jax.random.normal(k, shape, f32)

    return {
        "x": jax.random.normal(ks[0], (BATCH, SEQ, D), f32),
        "mem": jax.random.normal(ks[1], (BATCH, MEM_LEN, D), f32),
        "norm_mix": gain(ks[2], (L, D)),
        "w_in": nrm(ks[3], (L, D, IN_WIDTH), D ** -0.5),
        "sgu_ln_g": gain(ks[4], (L, SGU_GROUPS, SGU_CH)),
        "sgu_ln_b": nrm(ks[5], (L, SGU_GROUPS, SGU_CH), 0.02),
        "sgu_w": nrm(ks[6], (L, SGU_GROUPS, CHUNK, CHUNK), CHUNK ** -0.5),
        "sgu_b": gain(ks[7], (L, SGU_GROUPS, CHUNK)),
        "lambda_q1": nrm(ks[8], (L, DIFF_QK_DIM), 0.1),
        "lambda_k1": nrm(ks[9], (L, DIFF_QK_DIM), 0.1),
        "lambda_q2": nrm(ks[10], (L, DIFF_QK_DIM), 0.1),
        "lambda_k2": nrm(ks[11], (L, DIFF_QK_DIM), 0.1),
        "diff_subln": gain(ks[12], (L, DIFF_V_DIM)),
        "w_out": nrm(ks[13], (L, MIX_WIDTH, D), MIX_WIDTH ** -0.5),
        "norm_xq": gain(ks[14], (L, D)),
        "norm_mem": gain(ks[15], (L, D)),
        "w_q_mem": nrm(ks[16], (L, D, MEM_WIDTH), D ** -0.5),
        "w_kv_mem": nrm(ks[17], (L, D, 2 * MEM_WIDTH), D ** -0.5),
        "w_o_mem": nrm(ks[18], (L, MEM_WIDTH, D), MEM_WIDTH ** -0.5),
        "norm_ffn": gain(ks[19], (L, D)),
        "w_router_group": nrm(ks[20], (L, D, N_GROUPS), D ** -0.5),
        "b_router_group": nrm(ks[21], (L, N_GROUPS), 0.01),
        "w_router_expert": nrm(ks[22], (L, D, N_EXPERTS), D ** -0.5),
        "b_router_expert": nrm(ks[23], (L, N_EXPERTS), 0.01),
        "w_gate": nrm(ks[24], (L, N_EXPERTS, D, D_EXPERT), D ** -0.5),
        "w_up": nrm(ks[25], (L, N_EXPERTS, D, D_EXPERT), D ** -0.5),
        "w_down": nrm(ks[26], (L, N_EXPERTS, D_EXPERT, D), D_EXPERT ** -0.5),
        "norm_final": gain(ks[27], (D,)),
    }


def reference(x, mem, norm_mix, w_in, sgu_ln_g, sgu_ln_b, sgu_w, sgu_b, lambda_q1, lambda_k1, lambda_q2,
              lambda_k2, diff_subln, w_out, norm_xq, norm_mem, w_q_mem, w_kv_mem, w_o_mem, norm_ffn,
              w_router_group, b_router_group, w_router_expert, b_router_expert, w_gate, w_up, w_down,
              norm_final):
    Bn, Sn, _ = x.shape
    cuts = [SGU_WIDTH, 2 * SGU_WIDTH, 2 * SGU_WIDTH + DIFF_QK_WIDTH, 2 * SGU_WIDTH + 2 * DIFF_QK_WIDTH]
    for l in range(DEPTH):
        lambda_init = 0.8 - 0.6 * math.exp(-0.3 * l)
        h = rms_norm(x, norm_mix[l])
        z = h @ w_in[l]
        zu, zv, zq, zk, zval = jnp.split(z, cuts, axis=-1)
        u = jax.nn.gelu(zu, approximate=False).reshape(Bn, Sn, SGU_GROUPS, SGU_CH)
        v = jax.nn.gelu(zv, approximate=False).reshape(Bn, Sn, SGU_GROUPS, SGU_CH)
        a_out = chunked_sgu(u, v, sgu_ln_g[l], sgu_ln_b[l], sgu_w[l], sgu_b[l]).reshape(Bn, Sn, SGU_WIDTH)
        q = zq.reshape(Bn, Sn, DIFF_HEADS, 2, DIFF_QK_DIM).transpose(0, 2, 3, 1, 4) * (DIFF_QK_DIM ** -0.5)
        k = zk.reshape(Bn, Sn, DIFF_HEADS, 2, DIFF_QK_DIM).transpose(0, 2, 3, 1, 4)
        vv = zval.reshape(Bn, Sn, DIFF_HEADS, DIFF_V_DIM).transpose(0, 2, 1, 3)
        lam = (jnp.exp(jnp.sum(lambda_q1[l].astype(jnp.float32) * lambda_k1[l].astype(jnp.float32)))
               - jnp.exp(jnp.sum(lambda_q2[l].astype(jnp.float32) * lambda_k2[l].astype(jnp.float32)))
               + lambda_init)
        o = diff_attention(q, k, vv, lam)
        o = rms_norm(o, diff_subln[l]) * (1.0 - lambda_init)
        mixed = jnp.concatenate([a_out, o.reshape(Bn, Sn, DIFF_WIDTH)], axis=-1) @ w_out[l]
        x = x + mixed
        x = x + memory_attention(rms_norm(x, norm_xq[l]), rms_norm(mem, norm_mem[l]),
                                 w_q_mem[l], w_kv_mem[l], w_o_mem[l])
        x = x + hierarchical_moe(rms_norm(x, norm_ffn[l]), w_router_group[l], b_router_group[l],
                                 w_router_expert[l], b_router_expert[l], w_gate[l], w_up[l], w_down[l])
    return rms_norm(x, norm_final)
```

```python
import math
from contextlib import ExitStack

import numpy as np
import concourse.bass as bass
import concourse.mybir as mybir
from concourse.bass_utils import run_bass_kernel_spmd

F32 = mybir.dt.float32
BF16 = mybir.dt.bfloat16
I32 = mybir.dt.int32
U32 = mybir.dt.uint32
AF = mybir.ActivationFunctionType
ALU = mybir.AluOpType
AX = mybir.AxisListType

D = 1024
NCH = 8
IN_W = 2560
MEM = 256
NE = 32
DE = 512
RB = 256
RMS_EPS = 1e-6
LN_EPS = 1e-5
LAMBDA_INIT = 0.8 - 0.6 * math.exp(0.0)
SLOPES = [2.0 ** (-8.0 * (i + 1) / 4) for i in range(4)]


class KB:
    def __init__(self, nc, es):
        self.nc = nc
        self.es = es
        self.eng = {"pe": nc.tensor, "act": nc.scalar, "dve": nc.vector, "pool": nc.gpsimd, "sp": nc.sync}
        self.psem = {}
        self.cnt = {}
        for e in ("pe", "act", "dve", "pool"):
            self.psem[e] = es.enter_context(nc.semaphore("prog_" + e))
            self.cnt[e] = 0
        self.slots = {}
        self.next_slot = {}
        for q, n in (("sp", 12), ("pool", 12), ("act", 8)):
            self.slots[q] = [[es.enter_context(nc.semaphore("dma_%s_%d" % (q, i))), 0] for i in range(n)]
            self.next_slot[q] = 0
        self.seen = {e: {} for e in self.eng}
        self.emitted = {}
        self.lastw = {}
        self.reads = {}
        self.semobj = {}
        self.all_events = []

    def _wait(self, eng, ev):
        if ev is None:
            return
        sem, val, src = ev
        if src == "pe" and eng == "pe":
            return
        sid = id(sem)
        if self.seen[eng].get(sid, 0) >= val:
            return
        self.seen[eng][sid] = val
        assert self.emitted.get(sid, 0) >= val, ("wait on a value never produced", eng, src, val, self.emitted.get(sid, 0))
        self.eng[eng].wait_ge(sem, val)

    def _deps(self, eng, reads, writes):
        for k in reads:
            self._wait(eng, self.lastw.get(k))
        for k in writes:
            self._wait(eng, self.lastw.get(k))
            for ev in self.reads.get(k, ()):
                self._wait(eng, ev)

    def _record(self, ev, reads, writes):
        for k in reads:
            self.reads.setdefault(k, []).append(ev)
        for k in writes:
            self.lastw[k] = ev
            self.reads[k] = []

    def op(self, eng, emit, reads=(), writes=()):
        self._deps(eng, reads, writes)
        inst = emit(self.eng[eng])
        self.cnt[eng] += 1
        inst.then_inc(self.psem[eng], 1)
        self.emitted[id(self.psem[eng])] = self.cnt[eng]
        ev = (self.psem[eng], self.cnt[eng], eng)
        self._record(ev, reads, writes)
        return ev

    def pe(self, emits, reads=(), writes=()):
        self._deps("pe", reads, writes)
        inst = None
        for f in emits:
            inst = f(self.nc.tensor)
        self.cnt["pe"] += 1
        inst.then_inc(self.psem["pe"], 1)
        self.emitted[id(self.psem["pe"])] = self.cnt["pe"]
        ev = (self.psem["pe"], self.cnt["pe"], "pe")
        self._record(ev, reads, writes)
        return ev

    def dma(self, q, emit, reads=(), writes=()):
        uw = []
        for k in writes:
            if k.endswith("_s") or k == "out":
                self.uniq = getattr(self, "uniq", 0) + 1
                k = "%s#%d" % (k, self.uniq)
            uw.append(k)
        writes = uw
        i = self.next_slot[q]
        self.next_slot[q] = (i + 1) % len(self.slots[q])
        slot = self.slots[q][i]
        issuer = q
        if slot[1] > 0:
            self._wait(issuer, (slot[0], slot[1], "dma"))
        self._deps(issuer, reads, writes)
        inst = emit(self.eng[q])
        slot[1] += 16
        inst.then_inc(slot[0], 16)
        self.emitted[id(slot[0])] = slot[1]
        ev = (slot[0], slot[1], "dma")
        self._record(ev, reads, writes)
        return ev

    def sync_reads(self, eng, reads):
        for k in reads:
            self._wait(eng, self.lastw.get(k))

    def barrier(self):
        evs = [(self.psem[e], self.cnt[e], e + "_b") for e in self.psem if self.cnt[e] > 0]
        for q in self.slots:
            for s in self.slots[q]:
                if s[1] > 0:
                    evs.append((s[0], s[1], "dma"))
        for e in self.eng:
            for ev in evs:
                self._wait(e, ev)
        self.lastw = {}
        self.reads = {}


def bcast_rows(handle_ap, nrows, ncols, offset=0):
    return bass.AP(handle_ap.tensor, offset, [[0, nrows], [1, ncols]])


def build_program(S, debug=(), stages=("p1", "p2", "p3", "p4")):
    assert S % 512 == 0
    NT = S // 128
    NQ = S // 512
    A = 2 * S
    NBLK = A // RB + NE
    P_ROWS = NBLK * RB

    nc = bass.Bass("TRN2", target_bir_lowering=False)
    dt = nc.dram_tensor

    def din(name, shape, dtype=F32):
        return dt(name, list(shape), dtype, kind="ExternalInput")

    x_d = din("x", [S, D])
    mem_d = din("mem", [MEM, D])
    norm_mix_d = din("norm_mix", [1, D])
    w_in_d = din("w_in", [D, IN_W])
    sgu_ln_g_d = din("sgu_ln_g", [4, 128])
    sgu_ln_b_d = din("sgu_ln_b", [4, 128])
    sgu_w_d = din("sgu_w", [4, 128, 128])
    sgu_b_d = din("sgu_b", [1, 512])
    lam_d = din("lam4", [1, 256])
    subln_d = din("diff_subln", [1, 128])
    w_out_d = din("w_out", [D, D])
    norm_xq_d = din("norm_xq", [1, D])
    norm_mem_d = din("norm_mem", [1, D])
    w_q_d = din("w_q_mem", [D, D])
    w_kv_d = din("w_kv_mem", [D, 2 * D])
    w_o_d = din("w_o_mem", [D, D])
    norm_ffn_d = din("norm_ffn", [1, D])
    w_r_d = din("w_router", [D, 36])
    b_r_d = din("b_router", [1, 36])
    w_gate_d = din("w_gate", [NE * D, DE])
    w_up_d = din("w_up", [NE * D, DE])
    w_down_d = din("w_down", [NE * DE, D])
    norm_final_d = din("norm_final", [1, D])
    out_d = dt("out", [S, D], F32, kind="ExternalOutput")

    def scratch(name, shape, dtype):
        kind = "ExternalOutput" if name in debug else "Internal"
        return dt(name, list(shape), dtype, kind=kind)

    qT_s = scratch("qT_s", [4, 128, S], BF16)
    kT_s = scratch("kT_s", [4, 128, S], BF16)
    v_s = scratch("v_s", [S, 512], BF16)
    aT_s = scratch("aT_s", [4, 128, S], BF16)
    oT_s = scratch("oT_s", [4, 128, S], BF16)
    x2_s = scratch("x2_s", [S, D], F32)
    h3_s = scratch("h3_s", [S, D], BF16)
    xrows_s = scratch("xrows_s", [P_ROWS, D], BF16)
    yrows_s = scratch("yrows_s", [P_ROWS, D], F32)
    wall_s = scratch("wall_s", [NE * 128, 12288], BF16)

    with ExitStack() as es:
        kb = KB(nc, es)
        sb = lambda name, shape, dtype: es.enter_context(nc.sbuf_tensor("s_" + name, list(shape), dtype))

        ident_b = sb("ident_b", [128, 128], BF16)
        ident_f = sb("ident_f", [128, 128], F32)
        tri_b = sb("tri_b", [128, 128], BF16)
        ones_b = sb("ones_b", [128, 128], BF16)
        neghalf = sb("neghalf", [128, 8], F32)

        kb.op("pool", lambda e: e.memset(ones_b[:], 1.0), writes=["ones_b"])
        kb.op("pool", lambda e: e.memset(neghalf[:], -0.5), writes=["neghalf"])
        kb.op("pool", lambda e: e.memset(ident_f[:], 1.0), writes=["ident_f"])
        kb.op("pool", lambda e: e.affine_select(ident_f[:], ident_f[:], [[-1, 128]], ALU.is_equal, 0.0,
                                                 base=0, channel_multiplier=1),
              reads=["ident_f"], writes=["ident_f"])
        kb.op("pool", lambda e: e.tensor_copy(ident_b[:], ident_f[:]), reads=["ident_f"], writes=["ident_b"])
        kb.op("pool", lambda e: e.affine_select(tri_b[:], ones_b[:], [[1, 128]], ALU.is_ge, 0.0,
                                                 base=0, channel_multiplier=-1),
              reads=["ones_b"], writes=["tri_b"])

        def rsqrt_col(dst, src, n, key_dst, key_src):
            kb.op("pool", lambda e: e.tensor_tensor(dst, src, neghalf[:, 0:n], ALU.pow),
                  reads=[key_src, "neghalf"], writes=[key_dst])

        OHs = sb("OHs", [128, NT, 2, NE], BF16)
        RK = sb("RK", [128, NT, 2], F32)
        WT = sb("WT", [128, NT, 2], F32)
        BASE = sb("BASE", [128, NE], F32)
        G = dict(locals())
        if "p1" in stages:
            phase1(nc, kb, G)
        if "p2" in stages:
            phase2(nc, kb, G)
        if "p3" in stages:
            phase3(nc, kb, G)
            if "dbg_wt" in debug:
                dbg_wt = dt("dbg_wt", [128, NT * 2], F32, kind="ExternalOutput")
                dbg_rk = dt("dbg_rk", [128, NT * 2], F32, kind="ExternalOutput")
                dbg_oh = dt("dbg_oh", [128, NT * 2 * NE], F32, kind="ExternalOutput")
                kb.dma("sp", lambda e: e.dma_start(out=dbg_wt.ap(), in_=WT[:].rearrange("p t k -> p (t k)")), reads=["WT"])
                kb.dma("sp", lambda e: e.dma_start(out=dbg_rk.ap(), in_=RK[:].rearrange("p t k -> p (t k)")), reads=["RK"])
                kb.dma("sp", lambda e: e.dma_start(out=dbg_oh.ap(), in_=OHs[:].rearrange("p t k e -> p (t k e)")), reads=["OHs"])
        if "p4" in stages:
            phase4(nc, kb, G)
        kb.barrier()
    return nc


def phase1(nc, kb, g):
    S = g["S"]; NQ = g["NQ"]
    x_d = g["x_d"]; w_in_d = g["w_in_d"]
    ident_b = g["ident_b"]; ones_b = g["ones_b"]; neghalf = g["neghalf"]; ident_f = g["ident_f"]
    rsqrt_col = g["rsqrt_col"]
    with ExitStack() as es:
        sb = lambda name, shape, dtype: es.enter_context(nc.sbuf_tensor("s_" + name, list(shape), dtype))
        ps = lambda name, shape, dtype: es.enter_context(nc.psum_tensor("p_" + name, list(shape), dtype))
        w_in = sb("w_in", [128, NCH, IN_W], BF16)
        wstage = [sb("wstage%d" % i, [128, IN_W], F32) for i in range(2)]
        gmix = sb("gmix", [128, NCH], F32)
        lg = sb("lg", [128, 4], F32)
        lb = sb("lb", [128, 4], F32)
        wnat = sb("wnat", [128, 4, 128], F32)
        wmT = sb("wmT", [128, 4, 128], BF16)
        bs = sb("bs", [128, 512], F32)
        ct = sb("ct", [128, 512], F32)

        with nc.allow_non_contiguous_dma(reason="tiny param columns"):
            kb.dma("sp", lambda e: e.dma_start(out=gmix[:], in_=g["norm_mix_d"].ap().rearrange("o (c p) -> p (o c)", p=128)),
                   writes=["gmix"])
            kb.dma("sp", lambda e: e.dma_start(out=lg[:], in_=g["sgu_ln_g_d"].ap().rearrange("g c -> c g")), writes=["lg"])
            kb.dma("sp", lambda e: e.dma_start(out=lb[:], in_=g["sgu_ln_b_d"].ap().rearrange("g c -> c g")), writes=["lb"])
        kb.dma("sp", lambda e: e.dma_start(out=wnat[:], in_=g["sgu_w_d"].ap().rearrange("g t s -> t g s")), writes=["wnat"])
        kb.dma("sp", lambda e: e.dma_start(out=bs[:], in_=bcast_rows(g["sgu_b_d"].ap(), 128, 512)), writes=["bs"])

        for c in range(NCH):
            st = wstage[c % 2]
            kb.dma("sp", lambda e: e.dma_start(out=st[:], in_=w_in_d[c * 128:(c + 1) * 128, :]), writes=["wstage%d" % (c % 2)])
            kb.op("dve", lambda e: e.tensor_scalar(w_in[:, c, :], st[:], gmix[:, c:c + 1], None, ALU.mult),
                  reads=["wstage%d" % (c % 2), "gmix"], writes=["w_in"])

        pA = [ps("p1A%d" % i, [128, 512], F32) for i in range(4)]
        pT = [ps("p1T%d" % i, [128, 1024], BF16) for i in range(2)]
        pS = ps("p1S", [128, 512], F32)
        pW = ps("p1W", [128, 512], F32)

        kb.pe([(lambda e, gi=gi: e.transpose(pW[:, gi * 128:(gi + 1) * 128], wnat[:, gi, :], ident_f[:])) for gi in range(4)],
              reads=["wnat", "ident_f"], writes=["pW"])
        wtmp = sb("wtmp", [128, 512], F32)
        kb.op("dve", lambda e: e.tensor_copy(wtmp[:], pW[:]), reads=["pW"], writes=["wtmp"])
        kb.op("pool", lambda e: e.affine_select(wtmp[:].rearrange("p (g t) -> p g t", g=4), wtmp[:].rearrange("p (g t) -> p g t", g=4),
                                                 [[0, 4], [1, 128]], ALU.is_ge, 0.0, base=0, channel_multiplier=-1),
              reads=["wtmp"], writes=["wtmp"])
        kb.op("dve", lambda e: e.tensor_copy(wmT[:].rearrange("p g t -> p (g t)"), wtmp[:]), reads=["wtmp"], writes=["wmT"])
        kb.pe([lambda e: e.matmul(pW[:], ones_b[:], wmT[:].rearrange("p g t -> p (g t)"), start=True, stop=True)],
              reads=["ones_b", "wmT"], writes=["pW"])
        for gi in range(4):
            kb.op("dve", lambda e, gi=gi: e.scalar_tensor_tensor(ct[:, gi * 128:(gi + 1) * 128], pW[:, gi * 128:(gi + 1) * 128],
                                                               lb[:, gi:gi + 1], bs[:, gi * 128:(gi + 1) * 128], ALU.mult, ALU.add),
                  reads=["pW", "lb", "bs"], writes=["ct"])

        NB = 2
        xin = [sb("xin%d" % i, [128, D], F32) for i in range(4)]
        junk = sb("junk1", [128, D], BF16)
        hb = [sb("hb%d" % i, [128, D], BF16) for i in range(4)]
        st8 = [sb("st8_%d" % i, [128, 8], F32) for i in range(4)]
        hT = [sb("hT%d" % i, [128, NCH, 512], BF16) for i in range(NB)]
        uT = [sb("uT%d" % i, [128, 4, 512], BF16) for i in range(NB)]
        qo = [sb("qo%d" % i, [128, 4, 512], BF16) for i in range(NB)]
        ko = [sb("ko%d" % i, [128, 4, 512], BF16) for i in range(NB)]
        vo = [sb("vo%d" % i, [128, 512], BF16) for i in range(2)]
        vg = [sb("vg%d" % i, [128, 512], F32) for i in range(4)]
        vn = [sb("vn%d" % i, [128, 512], BF16) for i in range(4)]
        bst = [sb("bst%d" % i, [128, 4, 6], F32) for i in range(4)]
        mv = [sb("mv%d" % i, [128, 4, 2], F32) for i in range(4)]
        lrs = [sb("lrs%d" % i, [128, 8], F32) for i in range(4)]
        at = [sb("at%d" % i, [128, 4, 128], F32) for i in range(2)]
        ao = [sb("ao%d" % i, [128, 4, 512], BF16) for i in range(NB)]

        def a_pre(qb):
            for j in range(4):
                t = qb * 4 + j
                b = j
                kb.dma("act", lambda e: e.dma_start(out=xin[b][:], in_=x_d[t * 128:(t + 1) * 128, :]), writes=["xin%d" % b])
                kb.op("act", lambda e: e.activation(junk[:], xin[b][:], AF.Square, accum_out=st8[b][:, 0:1]),
                      reads=["xin%d" % b], writes=["junk1", "ss%d" % b])
                kb.op("dve", lambda e: e.tensor_scalar(st8[b][:, 1:2], st8[b][:, 0:1], 1.0 / D, RMS_EPS, ALU.mult, ALU.add),
                      reads=["ss%d" % b], writes=["ms%d" % b])
                rsqrt_col(st8[b][:, 2:3], st8[b][:, 1:2], 1, "rstd%d" % b, "ms%d" % b)
                kb.op("dve", lambda e: e.tensor_scalar(hb[b][:], xin[b][:], st8[b][:, 2:3], None, ALU.mult),
                      reads=["xin%d" % b, "rstd%d" % b], writes=["hb%d" % b])

        def a_T(qb):
            m = qb % NB
            for j in range(4):
                b = j
                pt = pT[j % 2]
                kb.pe([(lambda e, c=c: e.transpose(pt[:, c * 128:(c + 1) * 128], hb[b][:, c * 128:(c + 1) * 128], ident_b[:]))
                       for c in range(NCH)], reads=["hb%d" % b, "ident_b"], writes=["pT%d" % (j % 2)])
                if j % 2 == 0:
                    kb.op("act", lambda e: e.copy(hT[m][:, :, j * 128:(j + 1) * 128], pt[:].rearrange("p (c t) -> p c t", c=NCH)),
                          reads=["pT%d" % (j % 2)], writes=["hT%d_%d" % (m, j)])
                else:
                    kb.op("dve", lambda e: e.tensor_copy(hT[m][:, :, j * 128:(j + 1) * 128], pt[:].rearrange("p (c t) -> p c t", c=NCH)),
                          reads=["pT%d" % (j % 2)], writes=["hT%d_%d" % (m, j)])

        pi_ = {"n": 0}

        def nextp():
            i = pi_["n"] % 4
            pi_["n"] += 1
            return pA[i], "pA%d" % i

        def b_tok(qb):
            m = qb % NB
            hkeys = ["hT%d_%d" % (m, j) for j in range(4)]
            for j in range(4):
                b = j
                pz, pzk = nextp()
                kb.pe([(lambda e, c=c: e.matmul(pz[:], hT[m][:, c, j * 128:(j + 1) * 128], w_in[:, c, 512:1024], start=(c == 0), stop=(c == NCH - 1)))
                       for c in range(NCH)], reads=["w_in", hkeys[j]], writes=[pzk])
                kb.op("act", lambda e: e.activation(vg[b][:], pz[:], AF.Gelu), reads=[pzk], writes=["vg%d" % b])
                for gi in range(4):
                    kb.op("dve", lambda e, gi=gi: e.bn_stats(bst[b][:, gi, :], vg[b][:, gi * 128:(gi + 1) * 128]),
                          reads=["vg%d" % b], writes=["bst%d_%d" % (b, gi)])
                    kb.op("dve", lambda e, gi=gi: e.bn_aggr(mv[b][:, gi, :], bst[b][:, gi, :]),
                          reads=["bst%d_%d" % (b, gi)], writes=["mv%d_%d" % (b, gi)])
                mvk = ["mv%d_%d" % (b, gi) for gi in range(4)]
                kb.op("dve", lambda e: e.tensor_scalar(lrs[b][:, 0:4], mv[b][:, :, 1], LN_EPS, None, ALU.add), reads=mvk, writes=["lve%d" % b])
                rsqrt_col(lrs[b][:, 4:8], lrs[b][:, 0:4], 4, "lrs%d" % b, "lve%d" % b)
            for j in range(4):
                t = qb * 4 + j
                b = j
                mvk = ["mv%d_%d" % (b, gi) for gi in range(4)]
                pv, pvk = nextp()
                kb.pe([(lambda e, c=c: e.matmul(pv[:], hT[m][:, c, j * 128:(j + 1) * 128], w_in[:, c, 2048:2560], start=(c == 0), stop=(c == NCH - 1)))
                       for c in range(NCH)], reads=["w_in", hkeys[j]], writes=[pvk])
                kb.op("act", lambda e: e.copy(vo[j % 2][:], pv[:]), reads=[pvk], writes=["vo%d" % (j % 2)])
                kb.dma("sp", lambda e: e.dma_start(out=g["v_s"][t * 128:(t + 1) * 128, :], in_=vo[j % 2][:]), reads=["vo%d" % (j % 2)], writes=["v_s"])
                for gi in range(4):
                    kb.op("dve", lambda e, gi=gi: e.tensor_scalar(vn[b][:, gi * 128:(gi + 1) * 128], vg[b][:, gi * 128:(gi + 1) * 128],
                                                                  mv[b][:, gi, 0:1], lrs[b][:, 4 + gi:5 + gi], ALU.subtract, ALU.mult),
                          reads=["vg%d" % b, "lrs%d" % b] + mvk, writes=["vn%d" % b])

        def b_feat(qb):
            m = qb % NB
            hkeys = ["hT%d_%d" % (m, j) for j in range(4)]
            for kind, col0 in (("u", 0), ("q", 1024), ("k", 1536)):
                for gi in range(4):
                    pp, pk = nextp()
                    cs = col0 + gi * 128
                    kb.pe([(lambda e, c=c: e.matmul(pp[:], w_in[:, c, cs:cs + 128], hT[m][:, c, :], start=(c == 0), stop=(c == NCH - 1)))
                           for c in range(NCH)], reads=["w_in"] + hkeys, writes=[pk])
                    if kind == "u":
                        kb.op("act", lambda e: e.activation(uT[m][:, gi, :], pp[:], AF.Gelu), reads=[pk], writes=["uT%d_%d" % (m, gi)])
                    elif kind == "q":
                        kb.op("dve", lambda e: e.tensor_scalar(qo[m][:, gi, :], pp[:], 0.125, None, ALU.mult), reads=[pk], writes=["qo%d" % m])
                    else:
                        kb.op("act" if gi % 2 == 0 else "dve",
                              (lambda e: e.copy(ko[m][:, gi, :], pp[:])) if gi % 2 == 0 else (lambda e: e.tensor_copy(ko[m][:, gi, :], pp[:])),
                              reads=[pk], writes=["ko%d" % m])
            kb.dma("sp", lambda e: e.dma_start(out=g["qT_s"].ap()[:, :, qb * 512:(qb + 1) * 512].rearrange("h p t -> p h t"), in_=qo[m][:]),
                   reads=["qo%d" % m], writes=["qT_s"])
            kb.dma("sp", lambda e: e.dma_start(out=g["kT_s"].ap()[:, :, qb * 512:(qb + 1) * 512].rearrange("h p t -> p h t"), in_=ko[m][:]),
                   reads=["ko%d" % m], writes=["kT_s"])

        def b_sgu(qb):
            m = qb % NB
            for j in range(4):
                b = j
                kb.pe([(lambda e, gi=gi: e.matmul(pS[:, gi * 128:(gi + 1) * 128], vn[b][:, gi * 128:(gi + 1) * 128], wmT[:, gi, :], start=True, stop=True))
                       for gi in range(4)], reads=["vn%d" % b, "wmT"], writes=["pS"])
                for gi in range(4):
                    kb.op("dve", lambda e, gi=gi: e.scalar_tensor_tensor(at[j % 2][:, gi, :], pS[:, gi * 128:(gi + 1) * 128], lg[:, gi:gi + 1],
                                                                         ct[:, gi * 128:(gi + 1) * 128], ALU.mult, ALU.add),
                          reads=["pS", "lg", "ct"], writes=["at%d" % (j % 2)])
                kb.op("pool", lambda e: e.tensor_tensor(ao[m][:, :, j * 128:(j + 1) * 128], at[j % 2][:], uT[m][:, :, j * 128:(j + 1) * 128], ALU.mult),
                      reads=["at%d" % (j % 2)] + ["uT%d_%d" % (m, gi) for gi in range(4)], writes=["ao%d" % m])
            kb.dma("sp", lambda e: e.dma_start(out=g["aT_s"].ap()[:, :, qb * 512:(qb + 1) * 512].rearrange("h p t -> p h t"), in_=ao[m][:]),
                   reads=["ao%d" % m], writes=["aT_s"])

        a_pre(0)
        a_T(0)
        for qb in range(NQ):
            b_tok(qb)
            if qb + 1 < NQ:
                a_pre(qb + 1)
            b_feat(qb)
            if qb + 1 < NQ:
                a_T(qb + 1)
            b_sgu(qb)
        kb.barrier()


def phase2(nc, kb, g):
    S = g["S"]; NQ = g["NQ"]; NT = g["NT"]
    ones_b = g["ones_b"]; tri_b = g["tri_b"]; ident_b = g["ident_b"]; ident_f = g["ident_f"]
    NOFF = 4 * NQ
    off_min = -4 * (NQ - 1)
    with ExitStack() as es:
        sb = lambda name, shape, dtype: es.enter_context(nc.sbuf_tensor("s_" + name, list(shape), dtype))
        ps = lambda name, shape, dtype: es.enter_context(nc.psum_tensor("p_" + name, list(shape), dtype))
        v_all = sb("v_all", [128, NT, 512], BF16)
        qT = [sb("qT%d" % i, [128, S], BF16) for i in range(1)]
        kT = [sb("kT%d" % i, [128, S], BF16) for i in range(1)]
        cw = [sb("cw%d" % i, [128, 4096], BF16) for i in range(2)]
        wall_s = g["wall_s"]
        conv_steps = [(e_, mtx) for e_ in range(NE) for mtx in range(3)]
        conv_state = {"i": 0}

        def conv_store(i):
            e_, mtx = conv_steps[i]
            cb = i % 2
            kb.dma("sp", lambda e: e.dma_start(out=wall_s[e_ * 128:(e_ + 1) * 128, mtx * 4096:(mtx + 1) * 4096], in_=cw[cb][:]),
                   reads=["cw%d" % cb], writes=["wall_s"])

        def conv_step():
            i = conv_state["i"]
            if i > 0 and i <= len(conv_steps):
                conv_store(i - 1)
            if i < len(conv_steps):
                e_, mtx = conv_steps[i]
                cb = i % 2
                if mtx == 0:
                    src = g["w_gate_d"][e_ * D:(e_ + 1) * D, :].rearrange("(c p) f -> p c f", p=128)
                    dst = cw[cb][:].rearrange("p (c f) -> p c f", c=NCH)
                elif mtx == 1:
                    src = g["w_up_d"][e_ * D:(e_ + 1) * D, :].rearrange("(c p) f -> p c f", p=128)
                    dst = cw[cb][:].rearrange("p (c f) -> p c f", c=NCH)
                else:
                    src = g["w_down_d"][e_ * DE:(e_ + 1) * DE, :].rearrange("(c p) f -> p c f", p=128)
                    dst = cw[cb][:].rearrange("p (c f) -> p c f", c=4)
                kb.dma("pool", lambda e: e.dma_start(out=dst, in_=src), writes=["cw%d" % cb])
            conv_state["i"] = i + 1

        bias_i = sb("bias_i", [128, NOFF], I32)
        bias_f = sb("bias_f", [128, NOFF], F32)
        biasT = sb("biasT", [128, 4, NOFF], F32)
        lam4 = sb("lam4", [128, 256], F32)
        ltmp = sb("ltmp", [128, 128], F32)
        lsm = sb("lsm", [128, 8], F32)
        subg = sb("subg", [128, 2], F32)

        kb.dma("sp", lambda e: e.dma_start(out=v_all[:], in_=g["v_s"].ap().rearrange("(t p) d -> p t d", p=128)),
               reads=["v_s"], writes=["v_all"])
        kb.dma("sp", lambda e: e.dma_start(out=lam4[:], in_=bcast_rows(g["lam_d"].ap(), 128, 256)), writes=["lam4"])
        with nc.allow_non_contiguous_dma(reason="tiny param column"):
            kb.dma("sp", lambda e: e.dma_start(out=subg[:, 0:1], in_=g["subln_d"].ap().rearrange("o d -> d o")), writes=["subg"])
        kb.op("pool", lambda e: e.iota(bias_i[:], [[128, NOFF]], base=128 * off_min - 256, channel_multiplier=1), writes=["bias_i"])
        kb.op("dve", lambda e: e.tensor_copy(bias_f[:], bias_i[:]), reads=["bias_i"], writes=["bias_f"])
        for h in range(4):
            kb.op("dve", lambda e: e.tensor_scalar(biasT[:, h, :], bias_f[:], SLOPES[h], None, ALU.mult), reads=["bias_f"], writes=["biasT"])
        kb.op("dve", lambda e: e.tensor_scalar(subg[:, 1:2], subg[:, 0:1], 1.0 - LAMBDA_INIT, None, ALU.mult), reads=["subg"], writes=["subg2"])
        kb.op("dve", lambda e: e.tensor_tensor(ltmp[:, 0:64], lam4[:, 0:64], lam4[:, 64:128], ALU.mult), reads=["lam4"], writes=["ltmp0"])
        kb.op("dve", lambda e: e.tensor_tensor(ltmp[:, 64:128], lam4[:, 128:192], lam4[:, 192:256], ALU.mult), reads=["lam4"], writes=["ltmp1"])
        kb.op("dve", lambda e: e.reduce_sum(lsm[:, 0:2], ltmp[:].rearrange("p (a b) -> p a b", a=2), AX.X), reads=["ltmp0", "ltmp1"], writes=["lsm0"])
        kb.op("act", lambda e: e.activation(lsm[:, 2:4], lsm[:, 0:2], AF.Exp), reads=["lsm0"], writes=["lsm1"])
        kb.op("dve", lambda e: e.tensor_tensor(lsm[:, 4:5], lsm[:, 3:4], lsm[:, 2:3], ALU.subtract), reads=["lsm1"], writes=["lsm2"])
        kb.op("dve", lambda e: e.tensor_scalar(lsm[:, 5:6], lsm[:, 4:5], -LAMBDA_INIT, None, ALU.add), reads=["lsm2"], writes=["nlam"])
        nlam = lsm[:, 5:6]

        pS = [ps("p2S%d" % i, [128, 1024], F32) for i in range(2)]
        pO = [ps("p2O%d" % mp, [128, 512], F32) for mp in range(2)]
        pL = ps("p2L", [128, 512], F32)
        pX = ps("p2X", [128, 512], F32)
        NPB = 3
        pT = [sb("pT%d" % i, [128, 2, 512], BF16) for i in range(NPB)]
        ones_f = sb("ones_f", [128, 128], F32)
        kb.op("pool", lambda e: e.memset(ones_f[:], 1.0), writes=["ones_f"])
        negm = sb("negm", [128, 128], BF16)
        kb.op("dve", lambda e: e.tensor_scalar(negm[:], tri_b[:], -1.0, 30000.0, ALU.add, ALU.mult), reads=["tri_b"], writes=["negm"])
        sel01 = sb("sel01", [64, 2], F32)
        nl8 = sb("nl8", [128, 8], F32)
        kb.op("pool", lambda e: e.memset(sel01[:], 0.0), writes=["sel01"])
        kb.op("pool", lambda e: e.memset(sel01[0:1, 0:1], 1.0), reads=["sel01"], writes=["sel01"])
        kb.op("pool", lambda e: e.memset(sel01[32:33, 1:2], 1.0), reads=["sel01"], writes=["sel01"])
        kb.op("pool", lambda e: e.memset(nl8[:], 1.0), writes=["nl8"])
        for j_ in range(4):
            kb.op("dve", lambda e: e.tensor_copy(nl8[:, 2 * j_ + 1:2 * j_ + 2], lsm[:, 5:6]), reads=["nl8", "nlam"], writes=["nl8"])
        EP = 3
        O0s = [sb("O0s%d" % i, [128, 512], F32) for i in range(EP)]
        O1s = [sb("O1s%d" % i, [128, 512], F32) for i in range(EP)]
        Ls = [sb("Ls%d" % i, [64, 512], F32) for i in range(EP)]
        rc = [sb("rc%d" % i, [128, 8], F32) for i in range(EP)]
        dgr = [sb("dgr%d" % i, [128, 8, 128], F32) for i in range(EP)]
        osb = [sb("osb%d" % i, [128, 512], F32) for i in range(EP)]
        sqb = [sb("sqb%d" % i, [128, 512], BF16) for i in range(EP)]
        rs4 = [sb("rs4_%d" % i, [128, 8], F32) for i in range(EP)]
        dg = [sb("dg%d" % i, [128, 4, 128], F32) for i in range(EP)]
        ot = [sb("ot%d" % i, [128, 512], BF16) for i in range(EP)]

        pending = []
        epi = {"n": 0}

        def epilogue(h, qb, ui_now):
            p = epi["n"] % EP
            epi["n"] += 1
            sfx = "_%d" % p
            kb.op("dve", lambda e: e.tensor_copy(O0s[p][:], pO[0][:]), reads=["pO0"], writes=["O0s" + sfx])
            kb.op("act", lambda e: e.copy(O1s[p][:], pO[1][:]), reads=["pO1"], writes=["O1s" + sfx])
            kb.op("dve", lambda e: e.tensor_copy(Ls[p][:], pL[0:64, :]), reads=["pL"], writes=["Ls" + sfx])

            def b1():
                kb.pe([(lambda e, j=j: e.matmul(pX[:, 2 * j:2 * j + 2], Ls[p][:, j * 128:(j + 1) * 128], sel01[:], start=True, stop=True))
                       for j in range(4)], reads=["Ls" + sfx, "sel01"], writes=["pX"])
                kb.op("dve", lambda e: e.reciprocal(rc[p][:, 0:8], pX[:, 0:8]), reads=["pX"], writes=["rc" + sfx])
                kb.op("dve", lambda e: e.tensor_tensor(rc[p][:, 0:8], rc[p][:, 0:8], nl8[:], ALU.mult),
                      reads=["rc" + sfx, "nl8"], writes=["rc" + sfx])
                for i8 in range(8):
                    kb.op("dve", lambda e, i8=i8: e.tensor_scalar(dgr[p][:, i8, :], ident_f[:], rc[p][:, i8:i8 + 1], None, ALU.mult),
                          reads=["ident_f", "rc" + sfx], writes=["dgr" + sfx])

            def b2a():
                kb.pe([(lambda e, j=j: e.matmul(pX[:, j * 128:(j + 1) * 128], ones_f[:], dgr[p][:, 2 * j, :], start=True, stop=True))
                       for j in range(4)], reads=["ones_f", "dgr" + sfx], writes=["pX"])
                kb.op("dve", lambda e: e.tensor_tensor(O0s[p][:], O0s[p][:], pX[:], ALU.mult), reads=["O0s" + sfx, "pX"], writes=["O0s" + sfx])

            def b2b():
                kb.pe([(lambda e, j=j: e.matmul(pX[:, j * 128:(j + 1) * 128], ones_f[:], dgr[p][:, 2 * j + 1, :], start=True, stop=True))
                       for j in range(4)], reads=["ones_f", "dgr" + sfx], writes=["pX"])
                kb.op("dve", lambda e: e.tensor_tensor(O1s[p][:], O1s[p][:], pX[:], ALU.mult), reads=["O1s" + sfx, "pX"], writes=["O1s" + sfx])
                kb.op("dve", lambda e: e.tensor_tensor(osb[p][:], O0s[p][:], O1s[p][:], ALU.add), reads=["O0s" + sfx, "O1s" + sfx], writes=["osb" + sfx])
                kb.op("dve", lambda e: e.tensor_tensor(sqb[p][:], osb[p][:], osb[p][:], ALU.mult), reads=["osb" + sfx], writes=["sqb" + sfx])

            def b3():
                kb.pe([(lambda e, j=j: e.matmul(pX[:, j:j + 1], sqb[p][:, j * 128:(j + 1) * 128], ones_b[:, 0:1], start=True, stop=True))
                       for j in range(4)], reads=["ones_b", "sqb" + sfx], writes=["pX"])
                kb.op("dve", lambda e: e.tensor_scalar(rs4[p][:, 0:4], pX[:, 0:4], 1.0 / 128, RMS_EPS, ALU.mult, ALU.add), reads=["pX"], writes=["rs4a" + sfx])
                kb.op("pool", lambda e: e.tensor_tensor(rs4[p][:, 4:8], rs4[p][:, 0:4], g["neghalf"][:, 0:4], ALU.pow),
                      reads=["rs4a" + sfx, "neghalf"], writes=["rs4b" + sfx])
                for j4 in range(4):
                    kb.op("dve", lambda e, j4=j4: e.tensor_scalar(dg[p][:, j4, :], ident_f[:], rs4[p][:, 4 + j4:5 + j4], None, ALU.mult),
                          reads=["ident_f", "rs4b" + sfx], writes=["dg" + sfx])

            def b4():
                kb.pe([(lambda e, j=j: e.matmul(pX[:, j * 128:(j + 1) * 128], ones_f[:], dg[p][:, j, :], start=True, stop=True))
                       for j in range(4)], reads=["ones_f", "dg" + sfx], writes=["pX"])
                kb.op("dve", lambda e: e.scalar_tensor_tensor(ot[p][:], osb[p][:], subg[:, 1:2], pX[:], ALU.mult, ALU.mult),
                      reads=["osb" + sfx, "subg2", "pX"], writes=["ot" + sfx])
                kb.dma("sp", lambda e: e.dma_start(out=g["oT_s"][h][:, qb * 512:(qb + 1) * 512], in_=ot[p][:]), reads=["ot" + sfx], writes=["oT_s"])

            for dly, fn in ((2, b1), (5, b2a), (7, b2b), (11, b3), (14, b4)):
                pending.append((ui_now + dly, fn))

        def run_pending(ui_now, flush=False):
            n = 0
            pending.sort(key=lambda t: t[0])
            while pending and (flush or (pending[0][0] <= ui_now and n < 2)):
                pending.pop(0)[1]()
                n += 1

        ui = 0
        CHK = max(512, S // 4)
        NCHK = S // CHK
        CONV_EVERY = 12
        for h in range(4):
            hb = 0
            for cc in range(NCHK):
                kb.dma("sp", lambda e: e.dma_start(out=qT[hb][:, cc * CHK:(cc + 1) * CHK], in_=g["qT_s"][h][:, cc * CHK:(cc + 1) * CHK]),
                       reads=["qT_s"], writes=["q_c%d" % cc])
                kb.dma("sp", lambda e: e.dma_start(out=kT[hb][:, cc * CHK:(cc + 1) * CHK], in_=g["kT_s"][h][:, cc * CHK:(cc + 1) * CHK]),
                       reads=["kT_s"], writes=["k_c%d" % cc])

            def keep(qb, kk):
                return SLOPES[h] * (512 * qb - 128 * kk + 129) <= 124.0
            units = [(qb, kk) for qb in range(NQ) for kk in range(4 * qb + 4) if keep(qb, kk)]
            first_kk = {}
            for (qb_, kk_) in units:
                first_kk.setdefault(qb_, kk_)

            def qk(u, slot):
                qb, kk = u
                off = kk - 4 * qb
                c0 = max(0, off) * 128
                rd = ["q_c%d" % ((qb * 512) // CHK), "k_c%d" % ((kk * 128) // CHK)]
                ksl = slice(kk * 128, (kk + 1) * 128)
                if off < 0:
                    kb.pe([(lambda e, mp=mp: e.matmul(pS[slot][:, mp * 512:(mp + 1) * 512], kT[hb][64 * mp:64 * mp + 64, ksl],
                                                      qT[hb][64 * mp:64 * mp + 64, qb * 512:(qb + 1) * 512], start=True, stop=True))
                           for mp in range(2)], reads=rd, writes=["pS%d" % slot])
                    return
                em = []
                for mp in range(2):
                    em.append(lambda e, mp=mp: e.matmul(pS[slot][:, mp * 512 + c0:mp * 512 + c0 + 128], kT[hb][64 * mp:64 * mp + 64, ksl],
                                                        qT[hb][64 * mp:64 * mp + 64, qb * 512 + c0:qb * 512 + c0 + 128], start=True, stop=False))
                for mp in range(2):
                    em.append(lambda e, mp=mp: e.matmul(pS[slot][:, mp * 512 + c0:mp * 512 + c0 + 128], ident_b[:], negm[:], start=False, stop=True))
                if c0 + 128 < 512:
                    for mp in range(2):
                        em.append(lambda e, mp=mp: e.matmul(pS[slot][:, mp * 512 + c0 + 128:(mp + 1) * 512], kT[hb][64 * mp:64 * mp + 64, ksl],
                                                            qT[hb][64 * mp:64 * mp + 64, qb * 512 + c0 + 128:(qb + 1) * 512], start=True, stop=True))
                kb.pe(em, reads=rd + ["ident_b", "negm"], writes=["pS%d" % slot])

            qk(units[0], ui % 2)
            if len(units) > 1:
                qk(units[1], (ui + 1) % 2)
            for i, (qb, kk) in enumerate(units):
                slot = ui % 2
                pb = ui % NPB
                off = kk - 4 * qb
                c0 = max(0, off) * 128
                bcol = off - off_min
                last = (kk == 4 * qb + 3)
                first = (kk == first_kk[qb])
                kb.op("act", lambda e: e.activation(pT[pb][:, :, c0:512], pS[slot][:].rearrange("p (m q) -> p m q", m=2)[:, :, c0:512], AF.Exp,
                                                    bias=biasT[:, h, bcol:bcol + 1], scale=1.0),
                      reads=["pS%d" % slot, "biasT"], writes=["pT%d" % pb])
                if i + 2 < len(units):
                    qk(units[i + 2], slot)
                kb.pe([(lambda e, mp=mp: e.matmul(pO[mp][:, c0:512], v_all[:, kk, h * 128:(h + 1) * 128], pT[pb][:, mp, c0:512], start=first, stop=last))
                       for mp in range(2)] +
                      [(lambda e, mp=mp: e.matmul(pL[32 * mp:32 * mp + 32, c0:512], ones_b[:, 0:32], pT[pb][:, mp, c0:512], start=first, stop=last,
                                                  tile_position=(0, 32 * mp))) for mp in range(2)],
                      reads=["v_all", "ones_b", "pT%d" % pb], writes=["pO0", "pO1", "pL"])
                ui += 1
                if ui % CONV_EVERY == 0:
                    conv_step()
                if last:
                    epilogue(h, qb, ui)
                run_pending(ui)
        run_pending(ui, flush=True)
        while conv_state["i"] <= len(conv_steps):
            conv_step()
        kb.barrier()


def phase3(nc, kb, g):
    S = g["S"]; NQ = g["NQ"]; NT = g["NT"]
    ones_b = g["ones_b"]; tri_b = g["tri_b"]; ident_b = g["ident_b"]; ident_f = g["ident_f"]
    rsqrt_col = g["rsqrt_col"]
    OHs = g["OHs"]; RK = g["RK"]; WT = g["WT"]; BASE = g["BASE"]
    x_d = g["x_d"]
    with ExitStack() as es:
        sb = lambda name, shape, dtype: es.enter_context(nc.sbuf_tensor("s_" + name, list(shape), dtype))
        ps = lambda name, shape, dtype: es.enter_context(nc.psum_tensor("p_" + name, list(shape), dtype))
        w_out = sb("w_out", [128, NCH, D], BF16)
        w_q = sb("w_q", [128, NCH, D], BF16)
        w_o = sb("w_o", [128, NCH, D], BF16)
        kmT = sb("kmT", [128, 8, MEM], BF16)
        vm = sb("vm", [128, 2, D], BF16)
        w_r = sb("w_r", [128, NCH, 36], F32)
        b_r = sb("b_r", [128, 36], F32)
        gffn = sb("gffn", [128, D], F32)
        gcol = sb("gcol", [128, 2 * NCH], F32)

        pM = [ps("p3M%d" % i, [128, 512], F32) for i in range(2)]
        pSc = ps("p3Sc", [128, 1024], F32)
        pOm = [ps("p3Om%d" % i, [128, 512], F32) for i in range(2)]
        pLm = ps("p3Lm", [128, 512], F32)
        pTb = ps("p3T", [128, 1024], BF16)

        with nc.allow_non_contiguous_dma(reason="tiny param columns"):
            kb.dma("sp", lambda e: e.dma_start(out=gcol[:, 0:NCH], in_=g["norm_xq_d"].ap().rearrange("o (c p) -> p (o c)", p=128)), writes=["gcol0"])
            kb.dma("sp", lambda e: e.dma_start(out=gcol[:, NCH:2 * NCH], in_=g["norm_mem_d"].ap().rearrange("o (c p) -> p (o c)", p=128)), writes=["gcol1"])
        kb.dma("sp", lambda e: e.dma_start(out=w_r[:], in_=g["w_r_d"].ap().rearrange("(c p) n -> p c n", p=128)), writes=["w_r"])
        kb.dma("sp", lambda e: e.dma_start(out=b_r[:], in_=bcast_rows(g["b_r_d"].ap(), 128, 36)), writes=["b_r"])
        kb.dma("sp", lambda e: e.dma_start(out=gffn[:], in_=bcast_rows(g["norm_ffn_d"].ap(), 128, D)), writes=["gffn"])
        for c in range(NCH):
            kb.dma("pool", lambda e: e.dma_start(out=w_out[:, c, :], in_=g["w_out_d"][c * 128:(c + 1) * 128, :]), writes=["w_out"])
            kb.dma("pool", lambda e: e.dma_start(out=w_o[:, c, :], in_=g["w_o_d"][c * 128:(c + 1) * 128, :]), writes=["w_o"])
        kb.op("pool", lambda e: e.memset(BASE[:], 0.0), writes=["BASE"])

        with ExitStack() as es2:
            sb2 = lambda name, shape, dtype: es2.enter_context(nc.sbuf_tensor("s_" + name, list(shape), dtype))
            stg = [sb2("stg%d" % i, [128, 2 * D], F32) for i in range(2)]
            wkv = sb2("wkv", [128, NCH, 2 * D], BF16)
            memx = [sb2("memx%d" % i, [128, D], F32) for i in range(2)]
            memb = [sb2("memb%d" % i, [128, D], BF16) for i in range(2)]
            mjunk = sb2("mjunk", [128, D], BF16)
            mst = sb2("mst", [128, 8], F32)
            memT = sb2("memT", [128, NCH, MEM], BF16)
            for c in range(NCH):
                st = stg[c % 2]
                kb.dma("sp", lambda e: e.dma_start(out=st[:, 0:D], in_=g["w_q_d"][c * 128:(c + 1) * 128, :]), writes=["stg%d" % (c % 2)])
                kb.op("dve", lambda e: e.tensor_scalar(w_q[:, c, :], st[:, 0:D], gcol[:, c:c + 1], None, ALU.mult),
                      reads=["stg%d" % (c % 2), "gcol0"], writes=["w_q"])
            for c in range(NCH):
                st = stg[c % 2]
                kb.dma("sp", lambda e: e.dma_start(out=st[:], in_=g["w_kv_d"][c * 128:(c + 1) * 128, :]), writes=["stg%d" % (c % 2)])
                kb.op("dve", lambda e: e.tensor_scalar(wkv[:, c, :], st[:], gcol[:, NCH + c:NCH + c + 1], None, ALU.mult),
                      reads=["stg%d" % (c % 2), "gcol1"], writes=["wkv"])
            for mc in range(2):
                kb.dma("sp", lambda e: e.dma_start(out=memx[mc][:], in_=g["mem_d"][mc * 128:(mc + 1) * 128, :]), writes=["memx%d" % mc])
                kb.op("act", lambda e: e.activation(mjunk[:], memx[mc][:], AF.Square, accum_out=mst[:, 3 * mc:3 * mc + 1]),
                      reads=["memx%d" % mc], writes=["mjunk", "mss%d" % mc])
                kb.op("dve", lambda e: e.tensor_scalar(mst[:, 3 * mc + 1:3 * mc + 2], mst[:, 3 * mc:3 * mc + 1], 1.0 / D, RMS_EPS, ALU.mult, ALU.add),
                      reads=["mss%d" % mc], writes=["mms%d" % mc])
                rsqrt_col(mst[:, 3 * mc + 2:3 * mc + 3], mst[:, 3 * mc + 1:3 * mc + 2], 1, "mrs%d" % mc, "mms%d" % mc)
                kb.op("dve", lambda e: e.tensor_scalar(memb[mc][:], memx[mc][:], mst[:, 3 * mc + 2:3 * mc + 3], None, ALU.mult),
                      reads=["memx%d" % mc, "mrs%d" % mc], writes=["memb%d" % mc])
                kb.pe([(lambda e, c=c: e.transpose(pTb[:, c * 128:(c + 1) * 128], memb[mc][:, c * 128:(c + 1) * 128], ident_b[:]))
                       for c in range(NCH)], reads=["memb%d" % mc, "ident_b"], writes=["pTb"])
                kb.op("dve", lambda e: e.tensor_copy(memT[:, :, mc * 128:(mc + 1) * 128], pTb[:].rearrange("p (c t) -> p c t", c=NCH)),
                      reads=["pTb"], writes=["memT"])
            for oc in range(8):
                pp = pM[oc % 2]; pk = "pM%d" % (oc % 2)
                kb.pe([(lambda e, c=c: e.matmul(pp[:, 0:MEM], wkv[:, c, oc * 128:(oc + 1) * 128], memT[:, c, :], start=(c == 0), stop=(c == NCH - 1)))
                       for c in range(NCH)], reads=["wkv", "memT"], writes=[pk])
                kb.op("dve", lambda e: e.tensor_copy(kmT[:, oc, :], pp[:, 0:MEM]), reads=[pk], writes=["kmT"])
            for mc in range(2):
                for n in range(2):
                    pp = pOm[n]; pk = "pOm%d" % n
                    kb.pe([(lambda e, c=c: e.matmul(pp[:], memT[:, c, mc * 128:(mc + 1) * 128], wkv[:, c, D + n * 512:D + (n + 1) * 512],
                                                    start=(c == 0), stop=(c == NCH - 1))) for c in range(NCH)],
                          reads=["wkv", "memT"], writes=[pk])
                    kb.op("dve", lambda e: e.tensor_copy(vm[:, mc, n * 512:(n + 1) * 512], pp[:]), reads=[pk], writes=["vm"])
            kb.barrier()

        NB = 2
        act8 = [sb("act8_%d" % i, [128, 8, 512], BF16) for i in range(NB)]
        xs = [[sb("xs%d_%d" % (i, j), [128, D], F32) for j in range(4)] for i in range(NB)]
        junk = sb("junk3", [128, D], BF16)
        st8 = [sb("st83_%d" % i, [128, 8], F32) for i in range(4)]
        st9 = [sb("st93_%d" % i, [128, 8], F32) for i in range(4)]
        h2b = [sb("h2b%d" % i, [128, D], BF16) for i in range(4)]
        h2T = sb("h2T", [128, NCH, 512], BF16)
        qmT = sb("qmT", [128, 8, 512], BF16)
        pm = [[sb("pm%d_%d" % (i, mc), [128, 512], BF16) for mc in range(2)] for i in range(2)]
        Rm = [sb("Rm%d" % i, [128, 512], F32) for i in range(2)]
        om8 = sb("om8", [128, 8, 512], BF16)
        h3f = [sb("h3f%d" % i, [128, D], F32) for i in range(4)]
        h3b = [sb("h3b%d" % i, [128, D], BF16) for i in range(2)]
        h3T = [sb("h3T%d" % i, [128, NCH, 128], F32) for i in range(2)]
        rt = [sb("rt%d" % i, [128, 160], F32) for i in range(4)]
        ohsum = [sb("ohsum%d" % i, [128, NE], BF16) for i in range(4)]
        rtmp = [sb("rtmp%d" % i, [128, 3 * NE], F32) for i in range(2)]

        def rms_stats(xt, xk, st, sk):
            kb.op("act", lambda e: e.activation(junk[:], xt, AF.Square, accum_out=st[:, 0:1]), reads=[xk], writes=["junk3", sk + "ss"])
            kb.op("dve", lambda e: e.tensor_scalar(st[:, 1:2], st[:, 0:1], 1.0 / D, RMS_EPS, ALU.mult, ALU.add), reads=[sk + "ss"], writes=[sk + "ms"])
            rsqrt_col(st[:, 2:3], st[:, 1:2], 1, sk + "rs", sk + "ms")

        def loads(qb):
            m = qb % NB
            kb.dma("sp", lambda e: e.dma_start(out=act8[m][:, 0:4, :], in_=g["aT_s"].ap()[:, :, qb * 512:(qb + 1) * 512].rearrange("h p t -> p h t")),
                   reads=["aT_s"], writes=["act8_%d" % m])
            kb.dma("sp", lambda e: e.dma_start(out=act8[m][:, 4:8, :], in_=g["oT_s"].ap()[:, :, qb * 512:(qb + 1) * 512].rearrange("h p t -> p h t")),
                   reads=["oT_s"], writes=["act8_%d" % m])
            for j in range(4):
                t = qb * 4 + j
                kb.dma("sp", lambda e: e.dma_start(out=xs[m][j][:], in_=x_d[t * 128:(t + 1) * 128, :]), writes=["xs%d_%d" % (m, j)])

        import os
        NQ3 = int(os.environ.get("P3_NQ", NQ))
        router_q = []
        negone = sb("negone", [128, 2], F32)
        kb.op("pool", lambda e: e.memset(negone[:], -1.0), writes=["negone"])

        def router_some(n):
            for _ in range(n):
                if router_q:
                    f_, q_, j_ = router_q.pop(0); f_(q_, j_)

        loads(0)
        for qb in range(NQ3):
            m = qb % NB
            if qb + 1 < NQ3:
                loads(qb + 1)
            def mk_h2b(j):
                xt = xs[m][j]; xk = "xs%d_%d" % (m, j)
                st = st8[j]; sk = "st83_%d" % j
                kb.op("dve", lambda e: e.tensor_scalar(h2b[j][:], xt[:], st[:, 2:3], None, ALU.mult), reads=[xk, sk + "rs"], writes=["h2b%d" % j])

            for j in range(4):
                xt = xs[m][j]; xk = "xs%d_%d" % (m, j)
                for n in range(2):
                    kb.pe([(lambda e, c=c: e.matmul(pM[n][:], act8[m][:, c, j * 128:(j + 1) * 128], w_out[:, c, n * 512:(n + 1) * 512],
                                                    start=(c == 0), stop=(c == NCH - 1))) for c in range(NCH)],
                          reads=["act8_%d" % m, "w_out"], writes=["pM%d" % n])
                    kb.op("dve", lambda e: e.tensor_tensor(xt[:, n * 512:(n + 1) * 512], pM[n][:], xt[:, n * 512:(n + 1) * 512], ALU.add),
                          reads=["pM%d" % n, xk], writes=[xk])
                rms_stats(xt[:], xk, st8[j], "st83_%d" % j)
                if j >= 1:
                    mk_h2b(j - 1)
                router_some(2 if j < 2 else 1)
            mk_h2b(3)
            for j in range(4):
                hb_ = h2b[j]; hk = "h2b%d" % j
                kb.pe([(lambda e, c=c: e.transpose(pTb[:, c * 128:(c + 1) * 128], hb_[:, c * 128:(c + 1) * 128], ident_b[:]))
                       for c in range(NCH)], reads=[hk, "ident_b"], writes=["pTb"])
                kb.op("act", lambda e: e.copy(h2T[:, :, j * 128:(j + 1) * 128], pTb[:].rearrange("p (c t) -> p c t", c=NCH)),
                      reads=["pTb"], writes=["h2T_%d" % j])
                router_some(2 if j < 2 else 1)
            router_some(99)
            h2k = ["h2T_%d" % j for j in range(4)]
            def qm(oc):
                pp = pM[oc % 2]; pk = "pM%d" % (oc % 2)
                kb.pe([(lambda e, c=c: e.matmul(pp[:], w_q[:, c, oc * 128:(oc + 1) * 128], h2T[:, c, :], start=(c == 0), stop=(c == NCH - 1)))
                       for c in range(NCH)], reads=["w_q"] + h2k, writes=[pk])
                kb.op("act" if oc % 2 == 0 else "dve",
                      (lambda e: e.activation(qmT[:, oc, :], pp[:], AF.Copy, scale=1.0 / 16)) if oc % 2 == 0 else
                      (lambda e: e.tensor_scalar(qmT[:, oc, :], pp[:], 1.0 / 16, None, ALU.mult)),
                      reads=[pk], writes=["qmT_%d" % oc])

            qm(0); qm(1)
            for h in range(4):
                pb = h % 2
                for mc in range(2):
                    kb.pe([(lambda e, c2=c2: e.matmul(pSc[:, mc * 512:(mc + 1) * 512], kmT[:, h * 2 + c2, mc * 128:(mc + 1) * 128], qmT[:, h * 2 + c2, :],
                                                      start=(c2 == 0), stop=(c2 == 1))) for c2 in range(2)],
                          reads=["kmT", "qmT_%d" % (2 * h), "qmT_%d" % (2 * h + 1)], writes=["pSc%d" % mc])
                    kb.op("act", lambda e: e.activation(pm[pb][mc][:], pSc[:, mc * 512:(mc + 1) * 512], AF.Exp),
                          reads=["pSc%d" % mc], writes=["pm%d_%d" % (pb, mc)])
                if h + 1 < 4:
                    qm(2 * h + 2); qm(2 * h + 3)
                pmk = ["pm%d_%d" % (pb, mc) for mc in range(2)]
                kb.pe([(lambda e, mc=mc: e.matmul(pLm[:], ones_b[:], pm[pb][mc][:], start=(mc == 0), stop=(mc == 1))) for mc in range(2)],
                      reads=["ones_b"] + pmk, writes=["pLm", "pLm"])
                kb.op("dve", lambda e: e.reciprocal(Rm[pb][:], pLm[:]), reads=["pLm"], writes=["Rm%d" % pb])
                for c2 in range(2):
                    kb.pe([(lambda e, mc=mc: e.matmul(pOm[c2][:], vm[:, mc, h * 256 + c2 * 128:h * 256 + (c2 + 1) * 128], pm[pb][mc][:],
                                                      start=(mc == 0), stop=(mc == 1))) for mc in range(2)],
                          reads=["vm"] + pmk, writes=["pOm%d" % c2])
                    kb.op("dve", lambda e: e.tensor_tensor(om8[:, h * 2 + c2, :], pOm[c2][:], Rm[pb][:], ALU.mult),
                          reads=["pOm%d" % c2, "Rm%d" % pb], writes=["om8_%d" % (h * 2 + c2)])
            omk = ["om8_%d" % i for i in range(8)]
            for j in range(4):
                t = qb * 4 + j
                xt = xs[m][j]; xk = "xs%d_%d" % (m, j)
                for n in range(2):
                    kb.pe([(lambda e, c=c: e.matmul(pM[n][:], om8[:, c, j * 128:(j + 1) * 128], w_o[:, c, n * 512:(n + 1) * 512],
                                                    start=(c == 0), stop=(c == NCH - 1))) for c in range(NCH)],
                          reads=omk + ["w_o"], writes=["pM%d" % n])
                    kb.op("dve", lambda e: e.tensor_tensor(xt[:, n * 512:(n + 1) * 512], pM[n][:], xt[:, n * 512:(n + 1) * 512], ALU.add),
                          reads=["pM%d" % n, xk], writes=[xk])
                kb.dma("sp", lambda e: e.dma_start(out=g["x2_s"][t * 128:(t + 1) * 128, :], in_=xt[:]), reads=[xk], writes=["x2_s"])
                st = st9[j]; sk = "st93_%d" % j
                rms_stats(xt[:], xk, st, sk)
                hf = h3f[j]; hfk = "h3f%d" % j
                kb.op("dve", lambda e: e.scalar_tensor_tensor(hf[:], xt[:], st[:, 2:3], gffn[:], ALU.mult, ALU.mult),
                      reads=[xk, sk + "rs", "gffn"], writes=[hfk])
                kb.op("act", lambda e: e.copy(h3b[j % 2][:], hf[:]), reads=[hfk], writes=["h3b%d" % (j % 2)])
                kb.dma("sp", lambda e: e.dma_start(out=g["h3_s"][t * 128:(t + 1) * 128, :], in_=h3b[j % 2][:]), reads=["h3b%d" % (j % 2)], writes=["h3_s"])

            def r_T(qb, j):
                hf = h3f[j]; hfk = "h3f%d" % j
                r = j % 2
                pbank = pSc if r == 0 else None
                if r == 0:
                    kb.pe([(lambda e, c=c: e.transpose(pSc[:, c * 128:(c + 1) * 128], hf[:, c * 128:(c + 1) * 128], ident_f[:]))
                           for c in range(NCH)], reads=[hfk, "ident_f"], writes=["pSc0", "pSc1"])
                    kb.op("dve", lambda e: e.tensor_copy(h3T[r][:], pSc[:].rearrange("p (c t) -> p c t", c=NCH)),
                          reads=["pSc0", "pSc1"], writes=["h3T%d" % r])
                else:
                    kb.pe([(lambda e, c=c: e.transpose(pOm[c // 4][:, (c % 4) * 128:(c % 4 + 1) * 128], hf[:, c * 128:(c + 1) * 128], ident_f[:]))
                           for c in range(NCH)], reads=[hfk, "ident_f"], writes=["pOm0", "pOm1"])
                    kb.op("dve", lambda e: e.tensor_copy(h3T[r][:, 0:4, :], pOm[0][:].rearrange("p (c t) -> p c t", c=4)),
                          reads=["pOm0"], writes=["h3T%d" % r])
                    kb.op("act", lambda e: e.copy(h3T[r][:, 4:8, :], pOm[1][:].rearrange("p (c t) -> p c t", c=4)),
                          reads=["pOm1"], writes=["h3T%d" % r])

            def r_L(qb, j):
                t = qb * 4 + j
                r = j % 2
                kb.pe([(lambda e, c=c: e.matmul(pLm[:, 0:36], h3T[r][:, c, :], w_r[:, c, :], start=(c == 0), stop=(c == NCH - 1)))
                       for c in range(NCH)], reads=["h3T%d" % r, "w_r"], writes=["pLm"])
                R_ = rt[j]
                rk_ = "rt%d_" % j
                L = R_[:, 0:36]
                kb.op("dve", lambda e: e.tensor_tensor(L, pLm[:, 0:36], b_r[:], ALU.add), reads=["pLm", "b_r"], writes=[rk_ + "L"])
                kb.op("dve", lambda e: e.reduce_max(R_[:, 36:37], R_[:, 0:4], AX.X), reads=[rk_ + "L"], writes=[rk_ + "gmax"])
                kb.op("dve", lambda e: e.tensor_scalar(R_[:, 40:44], R_[:, 0:4], R_[:, 36:37], None, ALU.is_equal),
                      reads=[rk_ + "L", rk_ + "gmax"], writes=[rk_ + "ohg"])
                kb.op("dve", lambda e: e.tensor_scalar(R_[:, 37:38], R_[:, 36:37], -1.0, None, ALU.mult), reads=[rk_ + "gmax"], writes=[rk_ + "ngmax"])
                kb.op("act", lambda e: e.activation(R_[:, 44:48], R_[:, 0:4], AF.Exp, bias=R_[:, 37:38], scale=1.0, accum_out=R_[:, 38:39]),
                      reads=[rk_ + "L", rk_ + "ngmax"], writes=[rk_ + "eg", rk_ + "gsum"])
                kb.op("dve", lambda e: e.reciprocal(R_[:, 39:40], R_[:, 38:39]), reads=[rk_ + "gsum"], writes=[rk_ + "gate"])
                kb.op("dve", lambda e: e.tensor_scalar(R_[:, 48:52], R_[:, 40:44], -1.0, 1e30, ALU.add, ALU.mult), reads=[rk_ + "ohg"], writes=[rk_ + "pen"])
                for gi in range(4):
                    kb.op("dve", lambda e, gi=gi: e.tensor_scalar(R_[:, 56 + gi * 8:64 + gi * 8], R_[:, 4 + gi * 8:12 + gi * 8],
                                                                  R_[:, 48 + gi:49 + gi], None, ALU.add),
                          reads=[rk_ + "L", rk_ + "pen"], writes=[rk_ + "elm%d" % gi])
                elk = [rk_ + "elm%d" % gi for gi in range(4)]
                elm = R_[:, 56:88]
                kb.op("dve", lambda e: e.reduce_max(R_[:, 52:53], elm, AX.X), reads=elk, writes=[rk_ + "m1"])
                oh1 = OHs[:, t, 0, :]; oh2 = OHs[:, t, 1, :]
                kb.op("dve", lambda e: e.tensor_scalar(oh1, elm, R_[:, 52:53], None, ALU.is_equal), reads=elk + [rk_ + "m1"], writes=["OHs"])
                elm2 = R_[:, 88:120]
                kb.op("dve", lambda e: e.tensor_scalar(elm2, oh1, -1e30, None, ALU.mult), reads=["OHs"], writes=[rk_ + "elm2"])
                kb.op("dve", lambda e: e.tensor_tensor(elm2, elm2, elm, ALU.add), reads=elk + [rk_ + "elm2"], writes=[rk_ + "elm2"])
                kb.op("dve", lambda e: e.reduce_max(R_[:, 53:54], elm2, AX.X), reads=[rk_ + "elm2"], writes=[rk_ + "m2"])
                kb.op("dve", lambda e: e.tensor_scalar(oh2, elm2, R_[:, 53:54], None, ALU.is_equal), reads=[rk_ + "elm2", rk_ + "m2"], writes=["OHs"])
                kb.op("dve", lambda e: e.tensor_tensor(R_[:, 54:55], R_[:, 53:54], R_[:, 52:53], ALU.subtract),
                      reads=[rk_ + "m1", rk_ + "m2"], writes=[rk_ + "d"])
                kb.op("act", lambda e: e.activation(R_[:, 55:56], R_[:, 54:55], AF.Exp), reads=[rk_ + "d"], writes=[rk_ + "ed"])
                kb.op("dve", lambda e: e.tensor_scalar(R_[:, 120:121], R_[:, 55:56], 1.0, None, ALU.add), reads=[rk_ + "ed"], writes=[rk_ + "den"])
                kb.op("dve", lambda e: e.reciprocal(R_[:, 121:122], R_[:, 120:121]), reads=[rk_ + "den"], writes=[rk_ + "rden"])
                kb.op("dve", lambda e: e.tensor_tensor(WT[:, t, 0:1], R_[:, 121:122], R_[:, 39:40], ALU.mult),
                      reads=[rk_ + "rden", rk_ + "gate"], writes=["WT"])
                kb.op("dve", lambda e: e.tensor_tensor(WT[:, t, 1:2], R_[:, 39:40], WT[:, t, 0:1], ALU.subtract),
                      reads=[rk_ + "gate", "WT"], writes=["WT"])
                kb.op("dve", lambda e: e.tensor_tensor(ohsum[j][:], oh1, oh2, ALU.add), reads=["OHs"], writes=["ohsum%d" % j])

            def r_P(qb, j):
                t = qb * 4 + j
                r = j % 2
                kb.pe([lambda e: e.matmul(pLm[:, 64:64 + NE], tri_b[:], ohsum[j][:], start=True, stop=True),
                       lambda e: e.matmul(pLm[:, 64 + NE:64 + 2 * NE], ones_b[:], ohsum[j][:], start=True, stop=True)],
                      reads=["tri_b", "ones_b", "ohsum%d" % j], writes=["pLm"])
                T_ = rtmp[r]; tk = "rtmp%d_" % r
                kb.op("dve", lambda e: e.tensor_tensor(T_[:, 0:NE], pLm[:, 64:64 + NE], BASE[:], ALU.add), reads=["pLm", "BASE"], writes=[tk + "a"])
                for k in range(2):
                    kb.op("dve", lambda e, k=k: e.tensor_tensor(T_[:, (1 + k) * NE:(2 + k) * NE], OHs[:, t, k, :], T_[:, 0:NE], ALU.mult),
                          reads=["OHs", tk + "a"], writes=[tk + "b%d" % k])
                    kb.op("dve", lambda e, k=k: e.reduce_sum(RK[:, t, k:k + 1], T_[:, (1 + k) * NE:(2 + k) * NE], AX.X),
                          reads=[tk + "b%d" % k], writes=["RK"])
                kb.op("dve", lambda e: e.tensor_tensor(BASE[:], pLm[:, 64 + NE:64 + 2 * NE], BASE[:], ALU.add), reads=["pLm", "BASE"], writes=["BASE"])

            router_q.extend([(r_T, qb, 0), (r_T, qb, 1), (r_L, qb, 0), (r_T, qb, 2), (r_L, qb, 1), (r_T, qb, 3),
                             (r_L, qb, 2), (r_P, qb, 0), (r_L, qb, 3), (r_P, qb, 1), (r_P, qb, 2), (r_P, qb, 3)])
        while router_q:
            f_, q_, j_ = router_q.pop(0); f_(q_, j_)
        kb.barrier()


def phase4(nc, kb, g):
    S = g["S"]; NT = g["NT"]; NBLK = g["NBLK"]; P_ROWS = g["P_ROWS"]
    ident_b = g["ident_b"]; rsqrt_col = g["rsqrt_col"]
    OHs = g["OHs"]; RK = g["RK"]; WT = g["WT"]; BASE = g["BASE"]
    with ExitStack() as es:
        sb = lambda name, shape, dtype: es.enter_context(nc.sbuf_tensor("s_" + name, list(shape), dtype))
        ps = lambda name, shape, dtype: es.enter_context(nc.psum_tensor("p_" + name, list(shape), dtype))
        posi = sb("posi", [128, NT * 2], I32)
        idxw = sb("idxw", [128, NBLK], I32)
        gfin = sb("gfin", [128, D], F32)
        es_t = ExitStack()
        sbt = lambda name, shape, dtype: es_t.enter_context(nc.sbuf_tensor("s_" + name, list(shape), dtype))
        ci = sbt("ci", [128, NE], I32)
        cf = [sbt("cf%d" % i, [128, NE], F32) for i in range(4)]
        thr_i = sbt("thr_i", [128, NBLK], I32)
        thr = sbt("thr", [128, NBLK], F32)
        cmp_ = sbt("cmp", [128, NBLK, NE], F32)
        bef = sbt("bef", [128, 2 * NBLK], F32)
        ptmp = sbt("ptmp", [128, NT * 2, NE], F32)
        posf = sbt("posf", [128, NT * 2], F32)
        pidx_i = sbt("pidx_i", [128, 1], I32)
        pidx = sbt("pidx", [128, 1], F32)
        idxw_f = sbt("idxw_f", [128, NBLK], F32)
        hrow = [sbt("hrow%d" % i, [128, D], BF16) for i in range(6)]
        kb.dma("sp", lambda e: e.dma_start(out=gfin[:], in_=bcast_rows(g["norm_final_d"].ap(), 128, D)), writes=["gfin"])

        kb.op("dve", lambda e: e.tensor_scalar(cf[0][:], BASE[:], float(RB - 1), None, ALU.add), reads=["BASE"], writes=["cf0"])
        kb.op("dve", lambda e: e.tensor_copy(ci[:], cf[0][:]), reads=["cf0"], writes=["ci"])
        kb.op("dve", lambda e: e.tensor_single_scalar(ci[:], ci[:], 8, ALU.arith_shift_right), reads=["ci"], writes=["ci"])
        kb.op("dve", lambda e: e.tensor_single_scalar(ci[:], ci[:], 8, ALU.logical_shift_left), reads=["ci"], writes=["ci"])
        kb.op("dve", lambda e: e.tensor_copy(cf[1][:], ci[:]), reads=["ci"], writes=["cf1"])
        kb.op("dve", lambda e: e.tensor_copy(cf[2][:], cf[1][:]), reads=["cf1"], writes=["cf2"])
        cur, nxt = 2, 3
        for sh in (1, 2, 4, 8, 16):
            kb.op("dve", lambda e: e.tensor_copy(cf[nxt][:, 0:sh], cf[cur][:, 0:sh]), reads=["cf%d" % cur], writes=["cf%d" % nxt])
            kb.op("dve", lambda e: e.tensor_tensor(cf[nxt][:, sh:NE], cf[cur][:, sh:NE], cf[cur][:, 0:NE - sh], ALU.add),
                  reads=["cf%d" % cur], writes=["cf%d" % nxt])
            cur, nxt = nxt, cur
        pend = cf[cur]; pendk = "cf%d" % cur
        pst = cf[nxt]; pstk = "cf%d" % nxt
        kb.op("dve", lambda e: e.tensor_tensor(pst[:], pend[:], cf[1][:], ALU.subtract), reads=[pendk, "cf1"], writes=[pstk])
        kb.op("pool", lambda e: e.iota(thr_i[:], [[RB, NBLK]], base=0, channel_multiplier=0), writes=["thr_i"])
        kb.op("dve", lambda e: e.tensor_copy(thr[:], thr_i[:]), reads=["thr_i"], writes=["thr"])
        kb.op("dve", lambda e: e.tensor_tensor(cmp_[:], pend[:].unsqueeze(1).broadcast_to([128, NBLK, NE]),
                                               thr[:].unsqueeze(2).broadcast_to([128, NBLK, NE]), ALU.is_le),
              reads=[pendk, "thr"], writes=["cmp"])
        kb.op("dve", lambda e: e.reduce_sum(bef[:, 0:NBLK], cmp_[:], AX.X), reads=["cmp"], writes=["bef0"])
        kb.op("dve", lambda e: e.tensor_scalar(bef[:, 0:NBLK], bef[:, 0:NBLK], float(NE - 1), None, ALU.min),
              reads=["bef0"], writes=["bef0"])
        kb.op("dve", lambda e: e.tensor_tensor(ptmp[:], OHs[:].rearrange("p t k e -> p (t k) e"),
                                               pst[:].unsqueeze(1).broadcast_to([128, NT * 2, NE]), ALU.mult),
              reads=["OHs", pstk], writes=["ptmp"])
        kb.op("dve", lambda e: e.reduce_sum(posf[:], ptmp[:], AX.X), reads=["ptmp"], writes=["posf"])
        kb.op("dve", lambda e: e.scalar_tensor_tensor(posf[:], posf[:], -1.0, RK[:].rearrange("p t k -> p (t k)"), ALU.add, ALU.add),
              reads=["posf", "RK"], writes=["posf"])
        kb.op("dve", lambda e: e.tensor_copy(posi[:], posf[:]), reads=["posf"], writes=["posi"])

        kb.op("pool", lambda e: e.iota(pidx_i[:], [[0, 1]], base=0, channel_multiplier=1), writes=["pidx_i"])
        kb.op("dve", lambda e: e.tensor_copy(pidx[:], pidx_i[:]), reads=["pidx_i"], writes=["pidx"])
        kb.op("dve", lambda e: e.tensor_scalar(idxw_f[:], bef[:, 0:NBLK], 128.0, pidx[:, 0:1], ALU.mult, ALU.add),
              reads=["bef0", "pidx"], writes=["idxw_f"])
        kb.op("dve", lambda e: e.tensor_copy(idxw[:], idxw_f[:]), reads=["idxw_f"], writes=["idxw"])
        bc_rows = nc.gpsimd.to_reg(P_ROWS - 1)
        bc_w = nc.gpsimd.to_reg(NE * 128 - 1)
        for t in range(NT):
            hb_ = hrow[t % 6]; hk = "hrow%d" % (t % 6)
            kb.dma("sp", lambda e: e.dma_start(out=hb_[:], in_=g["h3_s"][t * 128:(t + 1) * 128, :]), reads=["h3_s"], writes=[hk])
            for k in range(2):
                kb.dma("pool", lambda e: e.indirect_dma_start(
                    out=g["xrows_s"][:, :], out_offset=bass.IndirectOffsetOnAxis(ap=posi[:, t * 2 + k:t * 2 + k + 1], axis=0),
                    in_=hb_[:], in_offset=None, bounds_check=bc_rows, oob_is_err=False),
                    reads=[hk, "posi"], writes=["xrows_s"])

        wall_s = g["wall_s"]
        kb.barrier()
        es_t.close()
        NWB = 3
        wbuf = [sb("wbuf%d" % i, [128, 12288], BF16) for i in range(NWB)]

        def load_weights(b):
            wb = b % NWB
            kb.dma("pool", lambda e: e.indirect_dma_start(
                out=wbuf[wb][:], out_offset=None, in_=wall_s[:, :],
                in_offset=bass.IndirectOffsetOnAxis(ap=idxw[:, b:b + 1], axis=0),
                bounds_check=bc_w, oob_is_err=False),
                reads=["wall_s", "idxw"], writes=["wbuf%d" % wb])

        kb.barrier()
        NXR = 6
        xr = [sb("xr%d" % i, [128, D], BF16) for i in range(NXR)]
        xT = [sb("xT%d" % i, [128, NCH, 128], BF16) for i in range(3)]
        sg = [sb("sg%d" % i, [128, DE], F32) for i in range(2)]
        ab = [sb("ab%d" % i, [128, DE], BF16) for i in range(2)]
        aT = [sb("aT%d" % i, [128, 4, 128], BF16) for i in range(2)]
        ysb = [sb("ysb%d" % i, [128, D], F32) for i in range(3)]
        pXT = [ps("p4XT%d" % i, [128, 1024], BF16) for i in range(2)]
        pG = ps("p4G", [128, 512], F32)
        pU = ps("p4U", [128, 512], F32)
        pAT = ps("p4AT", [128, 1024], BF16)
        pY = [ps("p4Y%d" % i, [128, 512], F32) for i in range(2)]
        NH = 2 * NBLK

        def st_load(i):
            r = i % NXR
            row0 = i * 128
            kb.dma("act", lambda e: e.dma_start(out=xr[r][:], in_=g["xrows_s"][row0:row0 + 128, :]), reads=["xrows_s"], writes=["xr%d" % r])

        def st_T(i):
            r = i % NXR; p = i % 2; x3 = i % 3
            kb.pe([(lambda e, c=c: e.transpose(pXT[p][:, c * 128:(c + 1) * 128], xr[r][:, c * 128:(c + 1) * 128], ident_b[:]))
                   for c in range(NCH)], reads=["xr%d" % r, "ident_b"], writes=["pXT%d" % p])
            if i % 2 == 0:
                kb.op("dve", lambda e: e.tensor_copy(xT[x3][:], pXT[p][:].rearrange("p (c t) -> p c t", c=NCH)), reads=["pXT%d" % p], writes=["xT%d" % x3])
            else:
                kb.op("act", lambda e: e.copy(xT[x3][:], pXT[p][:].rearrange("p (c t) -> p c t", c=NCH)), reads=["pXT%d" % p], writes=["xT%d" % x3])

        def st_GU(i):
            x3 = i % 3; r2 = i % 2; wb = (i // 2) % NWB
            emits = []
            for c in range(NCH):
                emits.append(lambda e, c=c: e.matmul(pG[:], xT[x3][:, c, :], wbuf[wb][:, c * 512:(c + 1) * 512], start=(c == 0), stop=(c == NCH - 1)))
                emits.append(lambda e, c=c: e.matmul(pU[:], xT[x3][:, c, :], wbuf[wb][:, 4096 + c * 512:4096 + (c + 1) * 512], start=(c == 0), stop=(c == NCH - 1)))
            kb.pe(emits, reads=["xT%d" % x3, "wbuf%d" % wb], writes=["pG", "pU"])
            kb.op("act", lambda e: e.activation(sg[r2][:], pG[:], AF.Silu), reads=["pG"], writes=["sg%d" % r2])
            kb.op("dve", lambda e: e.tensor_tensor(ab[r2][:], pU[:], sg[r2][:], ALU.mult), reads=["pU", "sg%d" % r2], writes=["ab%d" % r2])

        def st_TA(i):
            r2 = i % 2
            kb.pe([(lambda e, c=c: e.transpose(pAT[:, c * 128:(c + 1) * 128], ab[r2][:, c * 128:(c + 1) * 128], ident_b[:]))
                   for c in range(4)], reads=["ab%d" % r2, "ident_b"], writes=["pAT"])
            kb.op("dve", lambda e: e.tensor_copy(aT[r2][:], pAT[:, 0:512].rearrange("p (c t) -> p c t", c=4)), reads=["pAT"], writes=["aT%d" % r2])

        def st_DOWN(i):
            r2 = i % 2; y3 = i % 3; wb = (i // 2) % NWB
            row0 = i * 128
            for n in range(2):
                kb.pe([(lambda e, c=c: e.matmul(pY[n][:], aT[r2][:, c, :], wbuf[wb][:, 8192 + c * 1024 + n * 512:8192 + c * 1024 + (n + 1) * 512],
                                                start=(c == 0), stop=(c == 3))) for c in range(4)],
                      reads=["aT%d" % r2, "wbuf%d" % wb], writes=["pY%d" % n])
            kb.op("act", lambda e: e.copy(ysb[y3][:, 0:512], pY[0][:]), reads=["pY0"], writes=["ysb%d_0" % y3])
            kb.op("dve", lambda e: e.tensor_copy(ysb[y3][:, 512:1024], pY[1][:]), reads=["pY1"], writes=["ysb%d_1" % y3])
            kb.dma("sp", lambda e: e.dma_start(out=g["yrows_s"][row0:row0 + 128, :], in_=ysb[y3][:]),
                   reads=["ysb%d_0" % y3, "ysb%d_1" % y3], writes=["yrows_s"])

        for b_ in range(min(NWB, NBLK)):
            load_weights(b_)
        for i in range(min(5, NH)):
            st_load(i)
        st_T(0); st_T(1)
        st_GU(0)
        for s_ in range(NH):
            if s_ + 5 < NH:
                st_load(s_ + 5)
            if s_ + 2 < NH:
                st_T(s_ + 2)
            st_TA(s_)
            if s_ + 1 < NH:
                st_GU(s_ + 1)
            st_DOWN(s_)
            if s_ % 2 == 1 and (s_ // 2) + NWB < NBLK:
                load_weights(s_ // 2 + NWB)

        kb.barrier()
        NR = 3
        y1 = [sb("y1_%d" % i, [128, D], F32) for i in range(NR)]
        y2 = [sb("y2_%d" % i, [128, D], F32) for i in range(NR)]
        x2t = [sb("x2t%d" % i, [128, D], F32) for i in range(NR)]
        fjunk = sb("fjunk", [128, D], BF16)
        fst = [sb("fst%d" % i, [128, 8], F32) for i in range(NR)]
        fo = [sb("fo%d" % i, [128, D], F32) for i in range(NR)]

        def cb_load(t):
            r = t % NR
            for k, yy in ((0, y1), (1, y2)):
                kb.dma("pool", lambda e: e.indirect_dma_start(
                    out=yy[r][:], out_offset=None, in_=g["yrows_s"][:, :],
                    in_offset=bass.IndirectOffsetOnAxis(ap=posi[:, t * 2 + k:t * 2 + k + 1], axis=0),
                    bounds_check=bc_rows, oob_is_err=False),
                    reads=["yrows_s", "posi"], writes=["y%d_%d" % (k + 1, r)])
            kb.dma("sp", lambda e: e.dma_start(out=x2t[r][:], in_=g["x2_s"][t * 128:(t + 1) * 128, :]), reads=["x2_s"], writes=["x2t%d" % r])

        for t in range(min(2, NT)):
            cb_load(t)
        for t in range(NT):
            r = t % NR
            if t + 2 < NT:
                cb_load(t + 2)
            kb.op("dve", lambda e: e.scalar_tensor_tensor(x2t[r][:], y1[r][:], WT[:, t, 0:1], x2t[r][:], ALU.mult, ALU.add),
                  reads=["y1_%d" % r, "WT", "x2t%d" % r], writes=["x2t%d" % r])
            kb.op("dve", lambda e: e.scalar_tensor_tensor(x2t[r][:], y2[r][:], WT[:, t, 1:2], x2t[r][:], ALU.mult, ALU.add),
                  reads=["y2_%d" % r, "WT", "x2t%d" % r], writes=["x2t%d" % r])
            st = fst[r]; sk = "fst%d" % r
            kb.op("act", lambda e: e.activation(fjunk[:], x2t[r][:], AF.Square, accum_out=st[:, 0:1]), reads=["x2t%d" % r], writes=["fjunk", sk + "ss"])
            kb.op("dve", lambda e: e.tensor_scalar(st[:, 1:2], st[:, 0:1], 1.0 / D, RMS_EPS, ALU.mult, ALU.add), reads=[sk + "ss"], writes=[sk + "ms"])
            rsqrt_col(st[:, 2:3], st[:, 1:2], 1, sk + "rs", sk + "ms")
            kb.op("dve", lambda e: e.scalar_tensor_tensor(fo[r][:], x2t[r][:], st[:, 2:3], gfin[:], ALU.mult, ALU.mult),
                  reads=["x2t%d" % r, sk + "rs", "gfin"], writes=["fo%d" % r])
            kb.dma("sp", lambda e: e.dma_start(out=g["out_d"][t * 128:(t + 1) * 128, :], in_=fo[r][:]), reads=["fo%d" % r], writes=["out"])
        kb.barrier()


def core_inputs(inp, b):
    f = lambda a: np.ascontiguousarray(np.asarray(a, dtype=np.float32))
    return {
        "x": f(inp["x"][b]),
        "mem": f(inp["mem"][b]),
        "norm_mix": f(inp["norm_mix"]).reshape(1, D),
        "w_in": f(inp["w_in"][0]),
        "sgu_ln_g": f(inp["sgu_ln_g"][0]),
        "sgu_ln_b": f(inp["sgu_ln_b"][0]),
        "sgu_w": f(inp["sgu_w"][0]),
        "sgu_b": f(inp["sgu_b"][0]).reshape(1, 512),
        "lam4": f(np.concatenate([np.asarray(inp[k]).reshape(-1) for k in
                                  ("lambda_q1", "lambda_k1", "lambda_q2", "lambda_k2")])).reshape(1, 256),
        "diff_subln": f(inp["diff_subln"]).reshape(1, 128),
        "w_out": f(inp["w_out"][0]),
        "norm_xq": f(inp["norm_xq"]).reshape(1, D),
        "norm_mem": f(inp["norm_mem"]).reshape(1, D),
        "w_q_mem": f(inp["w_q_mem"][0]),
        "w_kv_mem": f(inp["w_kv_mem"][0]),
        "w_o_mem": f(inp["w_o_mem"][0]),
        "norm_ffn": f(inp["norm_ffn"]).reshape(1, D),
        "w_router": f(np.concatenate([np.asarray(inp["w_router_group"][0]), np.asarray(inp["w_router_expert"][0])], axis=1)),
        "b_router": f(np.concatenate([np.asarray(inp["b_router_group"]).reshape(-1),
                                      np.asarray(inp["b_router_expert"]).reshape(-1)])).reshape(1, 36),
        "w_gate": f(inp["w_gate"][0]).reshape(NE * D, DE),
        "w_up": f(inp["w_up"][0]).reshape(NE * D, DE),
        "w_down": f(inp["w_down"][0]).reshape(NE * DE, D),
        "norm_final": f(inp["norm_final"]).reshape(1, D),
    }


_PROGRAM_CACHE = {}


def kernel(**inputs):
    B, S, _ = np.asarray(inputs["x"]).shape
    if S not in _PROGRAM_CACHE:
        _PROGRAM_CACHE[S] = build_program(S)
    nc = _PROGRAM_CACHE[S]
    in_maps = [core_inputs(inputs, b) for b in range(B)]
    res = run_bass_kernel_spmd(nc, in_maps, core_ids=list(range(B)))
    out = np.stack([np.asarray(res.results[b]["out"], dtype=np.float32).reshape(S, D) for b in range(B)], axis=0)
    return out
```

```python
import math
from contextlib import ExitStack

import numpy as np
import concourse.bass as bass
import concourse.mybir as mybir
from concourse.bass_utils import run_bass_kernel_spmd

F32 = mybir.dt.float32
BF16 = mybir.dt.bfloat16
I32 = mybir.dt.int32
U32 = mybir.dt.uint32
AF = mybir.ActivationFunctionType
ALU = mybir.AluOpType
AX = mybir.AxisListType

D = 1024
NCH = 8
IN_W = 2560
MEM = 256
NE = 32
DE = 512
RB = 256
RMS_EPS = 1e-6
LN_EPS = 1e-5
LAMBDA_INIT = 0.8 - 0.6 * math.exp(0.0)
SLOPES = [2.0 ** (-8.0 * (i + 1) / 4) for i in range(4)]


class KB:
    def __init__(self, nc, es):
        self.nc = nc
        self.es = es
        self.eng = {"pe": nc.tensor, "act": nc.scalar, "dve": nc.vector, "pool": nc.gpsimd, "sp": nc.sync}
        self.psem = {}
        self.cnt = {}
        for e in ("pe", "act", "dve", "pool"):
            self.psem[e] = es.enter_context(nc.semaphore("prog_" + e))
            self.cnt[e] = 0
        self.slots = {}
        self.next_slot = {}
        for q, n in (("sp", 12), ("pool", 12), ("act", 8)):
            self.slots[q] = [[es.enter_context(nc.semaphore("dma_%s_%d" % (q, i))), 0] for i in range(n)]
            self.next_slot[q] = 0
        self.seen = {e: {} for e in self.eng}
        self.emitted = {}
        self.lastw = {}
        self.reads = {}
        self.semobj = {}
        self.all_events = []

    def _wait(self, eng, ev):
        if ev is None:
            return
        sem, val, src = ev
        if src == "pe" and eng == "pe":
            return
        sid = id(sem)
        if self.seen[eng].get(sid, 0) >= val:
            return
        self.seen[eng][sid] = val
        assert self.emitted.get(sid, 0) >= val, ("wait on a value never produced", eng, src, val, self.emitted.get(sid, 0))
        self.eng[eng].wait_ge(sem, val)

    def _deps(self, eng, reads, writes):
        for k in reads:
            self._wait(eng, self.lastw.get(k))
        for k in writes:
            self._wait(eng, self.lastw.get(k))
            for ev in self.reads.get(k, ()):
                self._wait(eng, ev)

    def _record(self, ev, reads, writes):
        for k in reads:
            self.reads.setdefault(k, []).append(ev)
        for k in writes:
            self.lastw[k] = ev
            self.reads[k] = []

    def op(self, eng, emit, reads=(), writes=()):
        self._deps(eng, reads, writes)
        inst = emit(self.eng[eng])
        self.cnt[eng] += 1
        inst.then_inc(self.psem[eng], 1)
        self.emitted[id(self.psem[eng])] = self.cnt[eng]
        ev = (self.psem[eng], self.cnt[eng], eng)
        self._record(ev, reads, writes)
        return ev

    def pe(self, emits, reads=(), writes=()):
        self._deps("pe", reads, writes)
        inst = None
        for f in emits:
            inst = f(self.nc.tensor)
        self.cnt["pe"] += 1
        inst.then_inc(self.psem["pe"], 1)
        self.emitted[id(self.psem["pe"])] = self.cnt["pe"]
        ev = (self.psem["pe"], self.cnt["pe"], "pe")
        self._record(ev, reads, writes)
        return ev

    def dma(self, q, emit, reads=(), writes=()):
        uw = []
        for k in writes:
            if k.endswith("_s") or k == "out":
                self.uniq = getattr(self, "uniq", 0) + 1
                k = "%s#%d" % (k, self.uniq)
            uw.append(k)
        writes = uw
        i = self.next_slot[q]
        self.next_slot[q] = (i + 1) % len(self.slots[q])
        slot = self.slots[q][i]
        issuer = q
        if slot[1] > 0:
            self._wait(issuer, (slot[0], slot[1], "dma"))
        self._deps(issuer, reads, writes)
        inst = emit(self.eng[q])
        slot[1] += 16
        inst.then_inc(slot[0], 16)
        self.emitted[id(slot[0])] = slot[1]
        ev = (slot[0], slot[1], "dma")
        self._record(ev, reads, writes)
        return ev

    def sync_reads(self, eng, reads):
        for k in reads:
            self._wait(eng, self.lastw.get(k))

    def barrier(self):
        evs = [(self.psem[e], self.cnt[e], e + "_b") for e in self.psem if self.cnt[e] > 0]
        for q in self.slots:
            for s in self.slots[q]:
                if s[1] > 0:
                    evs.append((s[0], s[1], "dma"))
        for e in self.eng:
            for ev in evs:
                self._wait(e, ev)
        self.lastw = {}
        self.reads = {}


def bcast_rows(handle_ap, nrows, ncols, offset=0):
    return bass.AP(handle_ap.tensor, offset, [[0, nrows], [1, ncols]])


def build_program(S, debug=(), stages=("p1", "p2", "p3", "p4")):
    assert S % 512 == 0
    NT = S // 128
    NQ = S // 512
    A = 2 * S
    NBLK = A // RB + NE
    P_ROWS = NBLK * RB

    nc = bass.Bass("TRN2", target_bir_lowering=False)
    dt = nc.dram_tensor

    def din(name, shape, dtype=F32):
        return dt(name, list(shape), dtype, kind="ExternalInput")

    x_d = din("x", [S, D])
    mem_d = din("mem", [MEM, D])
    norm_mix_d = din("norm_mix", [1, D])
    w_in_d = din("w_in", [D, IN_W])
    sgu_ln_g_d = din("sgu_ln_g", [4, 128])
    sgu_ln_b_d = din("sgu_ln_b", [4, 128])
    sgu_w_d = din("sgu_w", [4, 128, 128])
    sgu_b_d = din("sgu_b", [1, 512])
    lam_d = din("lam4", [1, 256])
    subln_d = din("diff_subln", [1, 128])
    w_out_d = din("w_out", [D, D])
    norm_xq_d = din("norm_xq", [1, D])
    norm_mem_d = din("norm_mem", [1, D])
    w_q_d = din("w_q_mem", [D, D])
    w_kv_d = din("w_kv_mem", [D, 2 * D])
    w_o_d = din("w_o_mem", [D, D])
    norm_ffn_d = din("norm_ffn", [1, D])
    w_r_d = din("w_router", [D, 36])
    b_r_d = din("b_router", [1, 36])
    w_gate_d = din("w_gate", [NE * D, DE])
    w_up_d = din("w_up", [NE * D, DE])
    w_down_d = din("w_down", [NE * DE, D])
    norm_final_d = din("norm_final", [1, D])
    out_d = dt("out", [S, D], F32, kind="ExternalOutput")

    def scratch(name, shape, dtype):
        kind = "ExternalOutput" if name in debug else "Internal"
        return dt(name, list(shape), dtype, kind=kind)

    qT_s = scratch("qT_s", [4, 128, S], BF16)
    kT_s = scratch("kT_s", [4, 128, S], BF16)
    v_s = scratch("v_s", [S, 512], BF16)
    aT_s = scratch("aT_s", [4, 128, S], BF16)
    oT_s = scratch("oT_s", [4, 128, S], BF16)
    x2_s = scratch("x2_s", [S, D], F32)
    h3_s = scratch("h3_s", [S, D], BF16)
    xrows_s = scratch("xrows_s", [P_ROWS, D], BF16)
    yrows_s = scratch("yrows_s", [P_ROWS, D], F32)
    wall_s = scratch("wall_s", [NE * 128, 12288], BF16)

    with ExitStack() as es:
        kb = KB(nc, es)
        sb = lambda name, shape, dtype: es.enter_context(nc.sbuf_tensor("s_" + name, list(shape), dtype))

        ident_b = sb("ident_b", [128, 128], BF16)
        ident_f = sb("ident_f", [128, 128], F32)
        tri_b = sb("tri_b", [128, 128], BF16)
        ones_b = sb("ones_b", [128, 128], BF16)
        neghalf = sb("neghalf", [128, 8], F32)

        kb.op("pool", lambda e: e.memset(ones_b[:], 1.0), writes=["ones_b"])
        kb.op("pool", lambda e: e.memset(neghalf[:], -0.5), writes=["neghalf"])
        kb.op("pool", lambda e: e.memset(ident_f[:], 1.0), writes=["ident_f"])
        kb.op("pool", lambda e: e.affine_select(ident_f[:], ident_f[:], [[-1, 128]], ALU.is_equal, 0.0,
                                                 base=0, channel_multiplier=1),
              reads=["ident_f"], writes=["ident_f"])
        kb.op("pool", lambda e: e.tensor_copy(ident_b[:], ident_f[:]), reads=["ident_f"], writes=["ident_b"])
        kb.op("pool", lambda e: e.affine_select(tri_b[:], ones_b[:], [[1, 128]], ALU.is_ge, 0.0,
                                                 base=0, channel_multiplier=-1),
              reads=["ones_b"], writes=["tri_b"])

        def rsqrt_col(dst, src, n, key_dst, key_src):
            kb.op("pool", lambda e: e.tensor_tensor(dst, src, neghalf[:, 0:n], ALU.pow),
                  reads=[key_src, "neghalf"], writes=[key_dst])

        OHs = sb("OHs", [128, NT, 2, NE], BF16)
        RK = sb("RK", [128, NT, 2], F32)
        WT = sb("WT", [128, NT, 2], F32)
        BASE = sb("BASE", [128, NE], F32)
        G = dict(locals())
        if "p1" in stages:
            phase1(nc, kb, G)
        if "p2" in stages:
            phase2(nc, kb, G)
        if "p3" in stages:
            phase3(nc, kb, G)
            if "dbg_wt" in debug:
                dbg_wt = dt("dbg_wt", [128, NT * 2], F32, kind="ExternalOutput")
                dbg_rk = dt("dbg_rk", [128, NT * 2], F32, kind="ExternalOutput")
                dbg_oh = dt("dbg_oh", [128, NT * 2 * NE], F32, kind="ExternalOutput")
                kb.dma("sp", lambda e: e.dma_start(out=dbg_wt.ap(), in_=WT[:].rearrange("p t k -> p (t k)")), reads=["WT"])
                kb.dma("sp", lambda e: e.dma_start(out=dbg_rk.ap(), in_=RK[:].rearrange("p t k -> p (t k)")), reads=["RK"])
                kb.dma("sp", lambda e: e.dma_start(out=dbg_oh.ap(), in_=OHs[:].rearrange("p t k e -> p (t k e)")), reads=["OHs"])
        if "p4" in stages:
            phase4(nc, kb, G)
        kb.barrier()
    return nc


def phase1(nc, kb, g):
    S = g["S"]; NQ = g["NQ"]
    x_d = g["x_d"]; w_in_d = g["w_in_d"]
    ident_b = g["ident_b"]; ones_b = g["ones_b"]; neghalf = g["neghalf"]; ident_f = g["ident_f"]
    rsqrt_col = g["rsqrt_col"]
    with ExitStack() as es:
        sb = lambda name, shape, dtype: es.enter_context(nc.sbuf_tensor("s_" + name, list(shape), dtype))
        ps = lambda name, shape, dtype: es.enter_context(nc.psum_tensor("p_" + name, list(shape), dtype))
        w_in = sb("w_in", [128, NCH, IN_W], BF16)
        wstage = [sb("wstage%d" % i, [128, IN_W], F32) for i in range(2)]
        gmix = sb("gmix", [128, NCH], F32)
        lg = sb("lg", [128, 4], F32)
        lb = sb("lb", [128, 4], F32)
        wnat = sb("wnat", [128, 4, 128], F32)
        wmT = sb("wmT", [128, 4, 128], BF16)
        bs = sb("bs", [128, 512], F32)
        ct = sb("ct", [128, 512], F32)

        with nc.allow_non_contiguous_dma(reason="tiny param columns"):
            kb.dma("sp", lambda e: e.dma_start(out=gmix[:], in_=g["norm_mix_d"].ap().rearrange("o (c p) -> p (o c)", p=128)),
                   writes=["gmix"])
            kb.dma("sp", lambda e: e.dma_start(out=lg[:], in_=g["sgu_ln_g_d"].ap().rearrange("g c -> c g")), writes=["lg"])
            kb.dma("sp", lambda e: e.dma_start(out=lb[:], in_=g["sgu_ln_b_d"].ap().rearrange("g c -> c g")), writes=["lb"])
        kb.dma("sp", lambda e: e.dma_start(out=wnat[:], in_=g["sgu_w_d"].ap().rearrange("g t s -> t g s")), writes=["wnat"])
        kb.dma("sp", lambda e: e.dma_start(out=bs[:], in_=bcast_rows(g["sgu_b_d"].ap(), 128, 512)), writes=["bs"])

        for c in range(NCH):
            st = wstage[c % 2]
            kb.dma("sp", lambda e: e.dma_start(out=st[:], in_=w_in_d[c * 128:(c + 1) * 128, :]), writes=["wstage%d" % (c % 2)])
            kb.op("dve", lambda e: e.tensor_scalar(w_in[:, c, :], st[:], gmix[:, c:c + 1], None, ALU.mult),
                  reads=["wstage%d" % (c % 2), "gmix"], writes=["w_in"])

        pA = [ps("p1A%d" % i, [128, 512], F32) for i in range(4)]
        pT = [ps("p1T%d" % i, [128, 1024], BF16) for i in range(2)]
        pS = ps("p1S", [128, 512], F32)
        pW = ps("p1W", [128, 512], F32)

        kb.pe([(lambda e, gi=gi: e.transpose(pW[:, gi * 128:(gi + 1) * 128], wnat[:, gi, :], ident_f[:])) for gi in range(4)],
              reads=["wnat", "ident_f"], writes=["pW"])
        wtmp = sb("wtmp", [128, 512], F32)
        kb.op("dve", lambda e: e.tensor_copy(wtmp[:], pW[:]), reads=["pW"], writes=["wtmp"])
        kb.op("pool", lambda e: e.affine_select(wtmp[:].rearrange("p (g t) -> p g t", g=4), wtmp[:].rearrange("p (g t) -> p g t", g=4),
                                                 [[0, 4], [1, 128]], ALU.is_ge, 0.0, base=0, channel_multiplier=-1),
              reads=["wtmp"], writes=["wtmp"])
        kb.op("dve", lambda e: e.tensor_copy(wmT[:].rearrange("p g t -> p (g t)"), wtmp[:]), reads=["wtmp"], writes=["wmT"])
        kb.pe([lambda e: e.matmul(pW[:], ones_b[:], wmT[:].rearrange("p g t -> p (g t)"), start=True, stop=True)],
              reads=["ones_b", "wmT"], writes=["pW"])
        for gi in range(4):
            kb.op("dve", lambda e, gi=gi: e.scalar_tensor_tensor(ct[:, gi * 128:(gi + 1) * 128], pW[:, gi * 128:(gi + 1) * 128],
                                                               lb[:, gi:gi + 1], bs[:, gi * 128:(gi + 1) * 128], ALU.mult, ALU.add),
                  reads=["pW", "lb", "bs"], writes=["ct"])

        NB = 2
        xin = [sb("xin%d" % i, [128, D], F32) for i in range(4)]
        junk = sb("junk1", [128, D], BF16)
        hb = [sb("hb%d" % i, [128, D], BF16) for i in range(4)]
        st8 = [sb("st8_%d" % i, [128, 8], F32) for i in range(4)]
        hT = [sb("hT%d" % i, [128, NCH, 512], BF16) for i in range(NB)]
        uT = [sb("uT%d" % i, [128, 4, 512], BF16) for i in range(NB)]
        qo = [sb("qo%d" % i, [128, 4, 512], BF16) for i in range(NB)]
        ko = [sb("ko%d" % i, [128, 4, 512], BF16) for i in range(NB)]
        vo = [sb("vo%d" % i, [128, 512], BF16) for i in range(2)]
        vg = [sb("vg%d" % i, [128, 512], F32) for i in range(4)]
        vn = [sb("vn%d" % i, [128, 512], BF16) for i in range(4)]
        bst = [sb("bst%d" % i, [128, 4, 6], F32) for i in range(4)]
        mv = [sb("mv%d" % i, [128, 4, 2], F32) for i in range(4)]
        lrs = [sb("lrs%d" % i, [128, 8], F32) for i in range(4)]
        at = [sb("at%d" % i, [128, 4, 128], F32) for i in range(2)]
        ao = [sb("ao%d" % i, [128, 4, 512], BF16) for i in range(NB)]

        def a_pre(qb):
            for j in range(4):
                t = qb * 4 + j
                b = j
                kb.dma("sp", lambda e: e.dma_start(out=xin[b][:], in_=x_d[t * 128:(t + 1) * 128, :]), writes=["xin%d" % b])
                kb.op("act", lambda e: e.activation(junk[:], xin[b][:], AF.Square, accum_out=st8[b][:, 0:1]),
                      reads=["xin%d" % b], writes=["junk1", "ss%d" % b])
                kb.op("dve", lambda e: e.tensor_scalar(st8[b][:, 1:2], st8[b][:, 0:1], 1.0 / D, RMS_EPS, ALU.mult, ALU.add),
                      reads=["ss%d" % b], writes=["ms%d" % b])
                rsqrt_col(st8[b][:, 2:3], st8[b][:, 1:2], 1, "rstd%d" % b, "ms%d" % b)
                kb.op("dve", lambda e: e.tensor_scalar(hb[b][:], xin[b][:], st8[b][:, 2:3], None, ALU.mult),
                      reads=["xin%d" % b, "rstd%d" % b], writes=["hb%d" % b])

        def a_T(qb):
            m = qb % NB
            for j in range(4):
                b = j
                pt = pT[j % 2]
                kb.pe([(lambda e, c=c: e.transpose(pt[:, c * 128:(c + 1) * 128], hb[b][:, c * 128:(c + 1) * 128], ident_b[:]))
                       for c in range(NCH)], reads=["hb%d" % b, "ident_b"], writes=["pT%d" % (j % 2)])
                if j % 2 == 0:
                    kb.op("act", lambda e: e.copy(hT[m][:, :, j * 128:(j + 1) * 128], pt[:].rearrange("p (c t) -> p c t", c=NCH)),
                          reads=["pT%d" % (j % 2)], writes=["hT%d_%d" % (m, j)])
                else:
                    kb.op("dve", lambda e: e.tensor_copy(hT[m][:, :, j * 128:(j + 1) * 128], pt[:].rearrange("p (c t) -> p c t", c=NCH)),
                          reads=["pT%d" % (j % 2)], writes=["hT%d_%d" % (m, j)])

        pi_ = {"n": 0}

        def nextp():
            i = pi_["n"] % 4
            pi_["n"] += 1
            return pA[i], "pA%d" % i

        def b_tok(qb):
            m = qb % NB
            hkeys = ["hT%d_%d" % (m, j) for j in range(4)]
            for j in range(4):
                b = j
                pz, pzk = nextp()
                kb.pe([(lambda e, c=c: e.matmul(pz[:], hT[m][:, c, j * 128:(j + 1) * 128], w_in[:, c, 512:1024], start=(c == 0), stop=(c == NCH - 1)))
                       for c in range(NCH)], reads=["w_in", hkeys[j]], writes=[pzk])
                kb.op("act", lambda e: e.activation(vg[b][:], pz[:], AF.Gelu), reads=[pzk], writes=["vg%d" % b])
                for gi in range(4):
                    kb.op("dve", lambda e, gi=gi: e.bn_stats(bst[b][:, gi, :], vg[b][:, gi * 128:(gi + 1) * 128]),
                          reads=["vg%d" % b], writes=["bst%d_%d" % (b, gi)])
                    kb.op("dve", lambda e, gi=gi: e.bn_aggr(mv[b][:, gi, :], bst[b][:, gi, :]),
                          reads=["bst%d_%d" % (b, gi)], writes=["mv%d_%d" % (b, gi)])
                mvk = ["mv%d_%d" % (b, gi) for gi in range(4)]
                kb.op("dve", lambda e: e.tensor_scalar(lrs[b][:, 0:4], mv[b][:, :, 1], LN_EPS, None, ALU.add), reads=mvk, writes=["lve%d" % b])
                rsqrt_col(lrs[b][:, 4:8], lrs[b][:, 0:4], 4, "lrs%d" % b, "lve%d" % b)
            for j in range(4):
                t = qb * 4 + j
                b = j
                mvk = ["mv%d_%d" % (b, gi) for gi in range(4)]
                pv, pvk = nextp()
                kb.pe([(lambda e, c=c: e.matmul(pv[:], hT[m][:, c, j * 128:(j + 1) * 128], w_in[:, c, 2048:2560], start=(c == 0), stop=(c == NCH - 1)))
                       for c in range(NCH)], reads=["w_in", hkeys[j]], writes=[pvk])
                kb.op("act", lambda e: e.copy(vo[j % 2][:], pv[:]), reads=[pvk], writes=["vo%d" % (j % 2)])
                kb.dma("sp", lambda e: e.dma_start(out=g["v_s"][t * 128:(t + 1) * 128, :], in_=vo[j % 2][:]), reads=["vo%d" % (j % 2)], writes=["v_s"])
                for gi in range(4):
                    kb.op("dve", lambda e, gi=gi: e.tensor_scalar(vn[b][:, gi * 128:(gi + 1) * 128], vg[b][:, gi * 128:(gi + 1) * 128],
                                                                  mv[b][:, gi, 0:1], lrs[b][:, 4 + gi:5 + gi], ALU.subtract, ALU.mult),
                          reads=["vg%d" % b, "lrs%d" % b] + mvk, writes=["vn%d" % b])

        def b_feat(qb):
            m = qb % NB
            hkeys = ["hT%d_%d" % (m, j) for j in range(4)]
            for kind, col0 in (("u", 0), ("q", 1024), ("k", 1536)):
                for gi in range(4):
                    pp, pk = nextp()
                    cs = col0 + gi * 128
                    kb.pe([(lambda e, c=c: e.matmul(pp[:], w_in[:, c, cs:cs + 128], hT[m][:, c, :], start=(c == 0), stop=(c == NCH - 1)))
                           for c in range(NCH)], reads=["w_in"] + hkeys, writes=[pk])
                    if kind == "u":
                        kb.op("act", lambda e: e.activation(uT[m][:, gi, :], pp[:], AF.Gelu), reads=[pk], writes=["uT%d_%d" % (m, gi)])
                    elif kind == "q":
                        kb.op("dve", lambda e: e.tensor_scalar(qo[m][:, gi, :], pp[:], 0.125, None, ALU.mult), reads=[pk], writes=["qo%d" % m])
                    else:
                        kb.op("act" if gi % 2 == 0 else "dve",
                              (lambda e: e.copy(ko[m][:, gi, :], pp[:])) if gi % 2 == 0 else (lambda e: e.tensor_copy(ko[m][:, gi, :], pp[:])),
                              reads=[pk], writes=["ko%d" % m])
            kb.dma("sp", lambda e: e.dma_start(out=g["qT_s"].ap()[:, :, qb * 512:(qb + 1) * 512].rearrange("h p t -> p h t"), in_=qo[m][:]),
                   reads=["qo%d" % m], writes=["qT_s"])
            kb.dma("sp", lambda e: e.dma_start(out=g["kT_s"].ap()[:, :, qb * 512:(qb + 1) * 512].rearrange("h p t -> p h t"), in_=ko[m][:]),
                   reads=["ko%d" % m], writes=["kT_s"])

        def b_sgu(qb):
            m = qb % NB
            for j in range(4):
                b = j
                kb.pe([(lambda e, gi=gi: e.matmul(pS[:, gi * 128:(gi + 1) * 128], vn[b][:, gi * 128:(gi + 1) * 128], wmT[:, gi, :], start=True, stop=True))
                       for gi in range(4)], reads=["vn%d" % b, "wmT"], writes=["pS"])
                for gi in range(4):
                    kb.op("dve", lambda e, gi=gi: e.scalar_tensor_tensor(at[j % 2][:, gi, :], pS[:, gi * 128:(gi + 1) * 128], lg[:, gi:gi + 1],
                                                                         ct[:, gi * 128:(gi + 1) * 128], ALU.mult, ALU.add),
                          reads=["pS", "lg", "ct"], writes=["at%d" % (j % 2)])
                kb.op("pool", lambda e: e.tensor_tensor(ao[m][:, :, j * 128:(j + 1) * 128], at[j % 2][:], uT[m][:, :, j * 128:(j + 1) * 128], ALU.mult),
                      reads=["at%d" % (j % 2)] + ["uT%d_%d" % (m, gi) for gi in range(4)], writes=["ao%d" % m])
            kb.dma("sp", lambda e: e.dma_start(out=g["aT_s"].ap()[:, :, qb * 512:(qb + 1) * 512].rearrange("h p t -> p h t"), in_=ao[m][:]),
                   reads=["ao%d" % m], writes=["aT_s"])

        a_pre(0)
        a_T(0)
        for qb in range(NQ):
            b_tok(qb)
            if qb + 1 < NQ:
                a_pre(qb + 1)
            b_feat(qb)
            if qb + 1 < NQ:
                a_T(qb + 1)
            b_sgu(qb)
        kb.barrier()


def phase2(nc, kb, g):
    S = g["S"]; NQ = g["NQ"]; NT = g["NT"]
    ones_b = g["ones_b"]; tri_b = g["tri_b"]; ident_b = g["ident_b"]; ident_f = g["ident_f"]
    NOFF = 4 * NQ
    off_min = -4 * (NQ - 1)
    with ExitStack() as es:
        sb = lambda name, shape, dtype: es.enter_context(nc.sbuf_tensor("s_" + name, list(shape), dtype))
        ps = lambda name, shape, dtype: es.enter_context(nc.psum_tensor("p_" + name, list(shape), dtype))
        v_all = sb("v_all", [128, NT, 512], BF16)
        qT = [sb("qT%d" % i, [128, S], BF16) for i in range(1)]
        kT = [sb("kT%d" % i, [128, S], BF16) for i in range(1)]
        cw = [sb("cw%d" % i, [128, 4096], BF16) for i in range(2)]
        wall_s = g["wall_s"]
        conv_steps = [(e_, mtx) for e_ in range(NE) for mtx in range(3)]
        conv_state = {"i": 0}

        def conv_store(i):
            e_, mtx = conv_steps[i]
            cb = i % 2
            kb.dma("sp", lambda e: e.dma_start(out=wall_s[e_ * 128:(e_ + 1) * 128, mtx * 4096:(mtx + 1) * 4096], in_=cw[cb][:]),
                   reads=["cw%d" % cb], writes=["wall_s"])

        def conv_step():
            i = conv_state["i"]
            if i > 0 and i <= len(conv_steps):
                conv_store(i - 1)
            if i < len(conv_steps):
                e_, mtx = conv_steps[i]
                cb = i % 2
                if mtx == 0:
                    src = g["w_gate_d"][e_ * D:(e_ + 1) * D, :].rearrange("(c p) f -> p c f", p=128)
                    dst = cw[cb][:].rearrange("p (c f) -> p c f", c=NCH)
                elif mtx == 1:
                    src = g["w_up_d"][e_ * D:(e_ + 1) * D, :].rearrange("(c p) f -> p c f", p=128)
                    dst = cw[cb][:].rearrange("p (c f) -> p c f", c=NCH)
                else:
                    src = g["w_down_d"][e_ * DE:(e_ + 1) * DE, :].rearrange("(c p) f -> p c f", p=128)
                    dst = cw[cb][:].rearrange("p (c f) -> p c f", c=4)
                kb.dma("pool", lambda e: e.dma_start(out=dst, in_=src), writes=["cw%d" % cb])
            conv_state["i"] = i + 1

        bias_i = sb("bias_i", [128, NOFF], I32)
        bias_f = sb("bias_f", [128, NOFF], F32)
        biasT = sb("biasT", [128, 4, NOFF], F32)
        lam4 = sb("lam4", [128, 256], F32)
        ltmp = sb("ltmp", [128, 128], F32)
        lsm = sb("lsm", [128, 8], F32)
        subg = sb("subg", [128, 2], F32)

        kb.dma("sp", lambda e: e.dma_start(out=v_all[:], in_=g["v_s"].ap().rearrange("(t p) d -> p t d", p=128)),
               reads=["v_s"], writes=["v_all"])
        kb.dma("sp", lambda e: e.dma_start(out=lam4[:], in_=bcast_rows(g["lam_d"].ap(), 128, 256)), writes=["lam4"])
        with nc.allow_non_contiguous_dma(reason="tiny param column"):
            kb.dma("sp", lambda e: e.dma_start(out=subg[:, 0:1], in_=g["subln_d"].ap().rearrange("o d -> d o")), writes=["subg"])
        kb.op("pool", lambda e: e.iota(bias_i[:], [[128, NOFF]], base=128 * off_min - 256, channel_multiplier=1), writes=["bias_i"])
        kb.op("dve", lambda e: e.tensor_copy(bias_f[:], bias_i[:]), reads=["bias_i"], writes=["bias_f"])
        for h in range(4):
            kb.op("dve", lambda e: e.tensor_scalar(biasT[:, h, :], bias_f[:], SLOPES[h], None, ALU.mult), reads=["bias_f"], writes=["biasT"])
        kb.op("dve", lambda e: e.tensor_scalar(subg[:, 1:2], subg[:, 0:1], 1.0 - LAMBDA_INIT, None, ALU.mult), reads=["subg"], writes=["subg2"])
        kb.op("dve", lambda e: e.tensor_tensor(ltmp[:, 0:64], lam4[:, 0:64], lam4[:, 64:128], ALU.mult), reads=["lam4"], writes=["ltmp0"])
        kb.op("dve", lambda e: e.tensor_tensor(ltmp[:, 64:128], lam4[:, 128:192], lam4[:, 192:256], ALU.mult), reads=["lam4"], writes=["ltmp1"])
        kb.op("dve", lambda e: e.reduce_sum(lsm[:, 0:2], ltmp[:].rearrange("p (a b) -> p a b", a=2), AX.X), reads=["ltmp0", "ltmp1"], writes=["lsm0"])
        kb.op("act", lambda e: e.activation(lsm[:, 2:4], lsm[:, 0:2], AF.Exp), reads=["lsm0"], writes=["lsm1"])
        kb.op("dve", lambda e: e.tensor_tensor(lsm[:, 4:5], lsm[:, 3:4], lsm[:, 2:3], ALU.subtract), reads=["lsm1"], writes=["lsm2"])
        kb.op("dve", lambda e: e.tensor_scalar(lsm[:, 5:6], lsm[:, 4:5], -LAMBDA_INIT, None, ALU.add), reads=["lsm2"], writes=["nlam"])
        nlam = lsm[:, 5:6]

        pS = [ps("p2S%d" % i, [128, 1024], F32) for i in range(2)]
        pO = [ps("p2O%d" % mp, [128, 512], F32) for mp in range(2)]
        pL = ps("p2L", [128, 512], F32)
        pX = ps("p2X", [128, 512], F32)
        NPB = 3
        pT = [sb("pT%d" % i, [128, 2, 512], BF16) for i in range(NPB)]
        ones_f = sb("ones_f", [128, 128], F32)
        kb.op("pool", lambda e: e.memset(ones_f[:], 1.0), writes=["ones_f"])
        negm = sb("negm", [128, 128], BF16)
        kb.op("dve", lambda e: e.tensor_scalar(negm[:], tri_b[:], -1.0, 30000.0, ALU.add, ALU.mult), reads=["tri_b"], writes=["negm"])
        sel01 = sb("sel01", [64, 2], F32)
        nl8 = sb("nl8", [128, 8], F32)
        kb.op("pool", lambda e: e.memset(sel01[:], 0.0), writes=["sel01"])
        kb.op("pool", lambda e: e.memset(sel01[0:1, 0:1], 1.0), reads=["sel01"], writes=["sel01"])
        kb.op("pool", lambda e: e.memset(sel01[32:33, 1:2], 1.0), reads=["sel01"], writes=["sel01"])
        kb.op("pool", lambda e: e.memset(nl8[:], 1.0), writes=["nl8"])
        for j_ in range(4):
            kb.op("dve", lambda e: e.tensor_copy(nl8[:, 2 * j_ + 1:2 * j_ + 2], lsm[:, 5:6]), reads=["nl8", "nlam"], writes=["nl8"])
        EP = 3
        O0s = [sb("O0s%d" % i, [128, 512], F32) for i in range(EP)]
        O1s = [sb("O1s%d" % i, [128, 512], F32) for i in range(EP)]
        Ls = [sb("Ls%d" % i, [64, 512], F32) for i in range(EP)]
        rc = [sb("rc%d" % i, [128, 8], F32) for i in range(EP)]
        dgr = [sb("dgr%d" % i, [128, 8, 128], F32) for i in range(EP)]
        osb = [sb("osb%d" % i, [128, 512], F32) for i in range(EP)]
        sqb = [sb("sqb%d" % i, [128, 512], BF16) for i in range(EP)]
        rs4 = [sb("rs4_%d" % i, [128, 8], F32) for i in range(EP)]
        dg = [sb("dg%d" % i, [128, 4, 128], F32) for i in range(EP)]
        ot = [sb("ot%d" % i, [128, 512], BF16) for i in range(EP)]

        pending = []
        epi = {"n": 0}

        def epilogue(h, qb, ui_now):
            p = epi["n"] % EP
            epi["n"] += 1
            sfx = "_%d" % p
            kb.op("dve", lambda e: e.tensor_copy(O0s[p][:], pO[0][:]), reads=["pO0"], writes=["O0s" + sfx])
            kb.op("act", lambda e: e.copy(O1s[p][:], pO[1][:]), reads=["pO1"], writes=["O1s" + sfx])
            kb.op("dve", lambda e: e.tensor_copy(Ls[p][:], pL[0:64, :]), reads=["pL"], writes=["Ls" + sfx])

            def b1():
                kb.pe([(lambda e, j=j: e.matmul(pX[:, 2 * j:2 * j + 2], Ls[p][:, j * 128:(j + 1) * 128], sel01[:], start=True, stop=True))
                       for j in range(4)], reads=["Ls" + sfx, "sel01"], writes=["pX"])
                kb.op("dve", lambda e: e.reciprocal(rc[p][:, 0:8], pX[:, 0:8]), reads=["pX"], writes=["rc" + sfx])
                kb.op("dve", lambda e: e.tensor_tensor(rc[p][:, 0:8], rc[p][:, 0:8], nl8[:], ALU.mult),
                      reads=["rc" + sfx, "nl8"], writes=["rc" + sfx])
                for i8 in range(8):
                    kb.op("dve", lambda e, i8=i8: e.tensor_scalar(dgr[p][:, i8, :], ident_f[:], rc[p][:, i8:i8 + 1], None, ALU.mult),
                          reads=["ident_f", "rc" + sfx], writes=["dgr" + sfx])

            def b2a():
                kb.pe([(lambda e, j=j: e.matmul(pX[:, j * 128:(j + 1) * 128], ones_f[:], dgr[p][:, 2 * j, :], start=True, stop=True))
                       for j in range(4)], reads=["ones_f", "dgr" + sfx], writes=["pX"])
                kb.op("dve", lambda e: e.tensor_tensor(O0s[p][:], O0s[p][:], pX[:], ALU.mult), reads=["O0s" + sfx, "pX"], writes=["O0s" + sfx])

            def b2b():
                kb.pe([(lambda e, j=j: e.matmul(pX[:, j * 128:(j + 1) * 128], ones_f[:], dgr[p][:, 2 * j + 1, :], start=True, stop=True))
                       for j in range(4)], reads=["ones_f", "dgr" + sfx], writes=["pX"])
                kb.op("dve", lambda e: e.tensor_tensor(O1s[p][:], O1s[p][:], pX[:], ALU.mult), reads=["O1s" + sfx, "pX"], writes=["O1s" + sfx])
                kb.op("dve", lambda e: e.tensor_tensor(osb[p][:], O0s[p][:], O1s[p][:], ALU.add), reads=["O0s" + sfx, "O1s" + sfx], writes=["osb" + sfx])
                kb.op("dve", lambda e: e.tensor_tensor(sqb[p][:], osb[p][:], osb[p][:], ALU.mult), reads=["osb" + sfx], writes=["sqb" + sfx])

            def b3():
                kb.pe([(lambda e, j=j: e.matmul(pX[:, j:j + 1], sqb[p][:, j * 128:(j + 1) * 128], ones_b[:, 0:1], start=True, stop=True))
                       for j in range(4)], reads=["ones_b", "sqb" + sfx], writes=["pX"])
                kb.op("dve", lambda e: e.tensor_scalar(rs4[p][:, 0:4], pX[:, 0:4], 1.0 / 128, RMS_EPS, ALU.mult, ALU.add), reads=["pX"], writes=["rs4a" + sfx])
                kb.op("pool", lambda e: e.tensor_tensor(rs4[p][:, 4:8], rs4[p][:, 0:4], g["neghalf"][:, 0:4], ALU.pow),
                      reads=["rs4a" + sfx, "neghalf"], writes=["rs4b" + sfx])
                for j4 in range(4):
                    kb.op("dve", lambda e, j4=j4: e.tensor_scalar(dg[p][:, j4, :], ident_f[:], rs4[p][:, 4 + j4:5 + j4], None, ALU.mult),
                          reads=["ident_f", "rs4b" + sfx], writes=["dg" + sfx])

            def b4():
                kb.pe([(lambda e, j=j: e.matmul(pX[:, j * 128:(j + 1) * 128], ones_f[:], dg[p][:, j, :], start=True, stop=True))
                       for j in range(4)], reads=["ones_f", "dg" + sfx], writes=["pX"])
                kb.op("dve", lambda e: e.scalar_tensor_tensor(ot[p][:], osb[p][:], subg[:, 1:2], pX[:], ALU.mult, ALU.mult),
                      reads=["osb" + sfx, "subg2", "pX"], writes=["ot" + sfx])
                kb.dma("sp", lambda e: e.dma_start(out=g["oT_s"][h][:, qb * 512:(qb + 1) * 512], in_=ot[p][:]), reads=["ot" + sfx], writes=["oT_s"])

            for dly, fn in ((2, b1), (5, b2a), (7, b2b), (11, b3), (14, b4)):
                pending.append((ui_now + dly, fn))

        def run_pending(ui_now, flush=False):
            n = 0
            pending.sort(key=lambda t: t[0])
            while pending and (flush or (pending[0][0] <= ui_now and n < 2)):
                pending.pop(0)[1]()
                n += 1

        ui = 0
        CHK = max(512, S // 4)
        NCHK = S // CHK
        CONV_EVERY = 15
        for h in range(4):
            hb = 0
            for cc in range(NCHK):
                kb.dma("sp", lambda e: e.dma_start(out=qT[hb][:, cc * CHK:(cc + 1) * CHK], in_=g["qT_s"][h][:, cc * CHK:(cc + 1) * CHK]),
                       reads=["qT_s"], writes=["q_c%d" % cc])
                kb.dma("sp", lambda e: e.dma_start(out=kT[hb][:, cc * CHK:(cc + 1) * CHK], in_=g["kT_s"][h][:, cc * CHK:(cc + 1) * CHK]),
                       reads=["kT_s"], writes=["k_c%d" % cc])

            def keep(qb, kk):
                return SLOPES[h] * (512 * qb - 128 * kk + 129) <= 124.0
            units = [(qb, kk) for qb in range(NQ) for kk in range(4 * qb + 4) if keep(qb, kk)]
            first_kk = {}
            for (qb_, kk_) in units:
                first_kk.setdefault(qb_, kk_)

            def qk(u, slot):
                qb, kk = u
                off = kk - 4 * qb
                c0 = max(0, off) * 128
                rd = ["q_c%d" % ((qb * 512) // CHK), "k_c%d" % ((kk * 128) // CHK)]
                ksl = slice(kk * 128, (kk + 1) * 128)
                if off < 0:
                    kb.pe([(lambda e, mp=mp: e.matmul(pS[slot][:, mp * 512:(mp + 1) * 512], kT[hb][64 * mp:64 * mp + 64, ksl],
                                                      qT[hb][64 * mp:64 * mp + 64, qb * 512:(qb + 1) * 512], start=True, stop=True))
                           for mp in range(2)], reads=rd, writes=["pS%d" % slot])
                    return
                em = []
                for mp in range(2):
                    em.append(lambda e, mp=mp: e.matmul(pS[slot][:, mp * 512 + c0:mp * 512 + c0 + 128], kT[hb][64 * mp:64 * mp + 64, ksl],
                                                        qT[hb][64 * mp:64 * mp + 64, qb * 512 + c0:qb * 512 + c0 + 128], start=True, stop=False))
                for mp in range(2):
                    em.append(lambda e, mp=mp: e.matmul(pS[slot][:, mp * 512 + c0:mp * 512 + c0 + 128], ident_b[:], negm[:], start=False, stop=True))
                if c0 + 128 < 512:
                    for mp in range(2):
                        em.append(lambda e, mp=mp: e.matmul(pS[slot][:, mp * 512 + c0 + 128:(mp + 1) * 512], kT[hb][64 * mp:64 * mp + 64, ksl],
                                                            qT[hb][64 * mp:64 * mp + 64, qb * 512 + c0 + 128:(qb + 1) * 512], start=True, stop=True))
                kb.pe(em, reads=rd + ["ident_b", "negm"], writes=["pS%d" % slot])

            qk(units[0], ui % 2)
            if len(units) > 1:
                qk(units[1], (ui + 1) % 2)
            for i, (qb, kk) in enumerate(units):
                slot = ui % 2
                pb = ui % NPB
                off = kk - 4 * qb
                c0 = max(0, off) * 128
                bcol = off - off_min
                last = (kk == 4 * qb + 3)
                first = (kk == first_kk[qb])
                kb.op("act", lambda e: e.activation(pT[pb][:, :, c0:512], pS[slot][:].rearrange("p (m q) -> p m q", m=2)[:, :, c0:512], AF.Exp,
                                                    bias=biasT[:, h, bcol:bcol + 1], scale=1.0),
                      reads=["pS%d" % slot, "biasT"], writes=["pT%d" % pb])
                if i + 2 < len(units):
                    qk(units[i + 2], slot)
                kb.pe([(lambda e, mp=mp: e.matmul(pO[mp][:, c0:512], v_all[:, kk, h * 128:(h + 1) * 128], pT[pb][:, mp, c0:512], start=first, stop=last))
                       for mp in range(2)] +
                      [(lambda e, mp=mp: e.matmul(pL[32 * mp:32 * mp + 32, c0:512], ones_b[:, 0:32], pT[pb][:, mp, c0:512], start=first, stop=last,
                                                  tile_position=(0, 32 * mp))) for mp in range(2)],
                      reads=["v_all", "ones_b", "pT%d" % pb], writes=["pO0", "pO1", "pL"])
                ui += 1
                if ui % CONV_EVERY == 0:
                    conv_step()
                if last:
                    epilogue(h, qb, ui)
                run_pending(ui)
        run_pending(ui, flush=True)
        while conv_state["i"] <= len(conv_steps):
            conv_step()
        kb.barrier()


def phase3(nc, kb, g):
    S = g["S"]; NQ = g["NQ"]; NT = g["NT"]
    ones_b = g["ones_b"]; tri_b = g["tri_b"]; ident_b = g["ident_b"]; ident_f = g["ident_f"]
    rsqrt_col = g["rsqrt_col"]
    OHs = g["OHs"]; RK = g["RK"]; WT = g["WT"]; BASE = g["BASE"]
    x_d = g["x_d"]
    with ExitStack() as es:
        sb = lambda name, shape, dtype: es.enter_context(nc.sbuf_tensor("s_" + name, list(shape), dtype))
        ps = lambda name, shape, dtype: es.enter_context(nc.psum_tensor("p_" + name, list(shape), dtype))
        w_out = sb("w_out", [128, NCH, D], BF16)
        w_q = sb("w_q", [128, NCH, D], BF16)
        w_o = sb("w_o", [128, NCH, D], BF16)
        kmT = sb("kmT", [128, 8, MEM], BF16)
        vm = sb("vm", [128, 2, D], BF16)
        w_r = sb("w_r", [128, NCH, 36], F32)
        b_r = sb("b_r", [128, 36], F32)
        gffn = sb("gffn", [128, D], F32)
        gcol = sb("gcol", [128, 2 * NCH], F32)

        pM = [ps("p3M%d" % i, [128, 512], F32) for i in range(2)]
        pSc = ps("p3Sc", [128, 1024], F32)
        pOm = [ps("p3Om%d" % i, [128, 512], F32) for i in range(2)]
        pLm = ps("p3Lm", [128, 512], F32)
        pTb = ps("p3T", [128, 1024], BF16)

        with nc.allow_non_contiguous_dma(reason="tiny param columns"):
            kb.dma("sp", lambda e: e.dma_start(out=gcol[:, 0:NCH], in_=g["norm_xq_d"].ap().rearrange("o (c p) -> p (o c)", p=128)), writes=["gcol0"])
            kb.dma("sp", lambda e: e.dma_start(out=gcol[:, NCH:2 * NCH], in_=g["norm_mem_d"].ap().rearrange("o (c p) -> p (o c)", p=128)), writes=["gcol1"])
        kb.dma("sp", lambda e: e.dma_start(out=w_r[:], in_=g["w_r_d"].ap().rearrange("(c p) n -> p c n", p=128)), writes=["w_r"])
        kb.dma("sp", lambda e: e.dma_start(out=b_r[:], in_=bcast_rows(g["b_r_d"].ap(), 128, 36)), writes=["b_r"])
        kb.dma("sp", lambda e: e.dma_start(out=gffn[:], in_=bcast_rows(g["norm_ffn_d"].ap(), 128, D)), writes=["gffn"])
        for c in range(NCH):
            kb.dma("pool", lambda e: e.dma_start(out=w_out[:, c, :], in_=g["w_out_d"][c * 128:(c + 1) * 128, :]), writes=["w_out"])
            kb.dma("pool", lambda e: e.dma_start(out=w_o[:, c, :], in_=g["w_o_d"][c * 128:(c + 1) * 128, :]), writes=["w_o"])
        kb.op("pool", lambda e: e.memset(BASE[:], 0.0), writes=["BASE"])

        with ExitStack() as es2:
            sb2 = lambda name, shape, dtype: es2.enter_context(nc.sbuf_tensor("s_" + name, list(shape), dtype))
            stg = [sb2("stg%d" % i, [128, 2 * D], F32) for i in range(2)]
            wkv = sb2("wkv", [128, NCH, 2 * D], BF16)
            memx = [sb2("memx%d" % i, [128, D], F32) for i in range(2)]
            memb = [sb2("memb%d" % i, [128, D], BF16) for i in range(2)]
            mjunk = sb2("mjunk", [128, D], BF16)
            mst = sb2("mst", [128, 8], F32)
            memT = sb2("memT", [128, NCH, MEM], BF16)
            for c in range(NCH):
                st = stg[c % 2]
                kb.dma("sp", lambda e: e.dma_start(out=st[:, 0:D], in_=g["w_q_d"][c * 128:(c + 1) * 128, :]), writes=["stg%d" % (c % 2)])
                kb.op("dve", lambda e: e.tensor_scalar(w_q[:, c, :], st[:, 0:D], gcol[:, c:c + 1], None, ALU.mult),
                      reads=["stg%d" % (c % 2), "gcol0"], writes=["w_q"])
            for c in range(NCH):
                st = stg[c % 2]
                kb.dma("sp", lambda e: e.dma_start(out=st[:], in_=g["w_kv_d"][c * 128:(c + 1) * 128, :]), writes=["stg%d" % (c % 2)])
                kb.op("dve", lambda e: e.tensor_scalar(wkv[:, c, :], st[:], gcol[:, NCH + c:NCH + c + 1], None, ALU.mult),
                      reads=["stg%d" % (c % 2), "gcol1"], writes=["wkv"])
            for mc in range(2):
                kb.dma("sp", lambda e: e.dma_start(out=memx[mc][:], in_=g["mem_d"][mc * 128:(mc + 1) * 128, :]), writes=["memx%d" % mc])
                kb.op("act", lambda e: e.activation(mjunk[:], memx[mc][:], AF.Square, accum_out=mst[:, 3 * mc:3 * mc + 1]),
                      reads=["memx%d" % mc], writes=["mjunk", "mss%d" % mc])
                kb.op("dve", lambda e: e.tensor_scalar(mst[:, 3 * mc + 1:3 * mc + 2], mst[:, 3 * mc:3 * mc + 1], 1.0 / D, RMS_EPS, ALU.mult, ALU.add),
                      reads=["mss%d" % mc], writes=["mms%d" % mc])
                rsqrt_col(mst[:, 3 * mc + 2:3 * mc + 3], mst[:, 3 * mc + 1:3 * mc + 2], 1, "mrs%d" % mc, "mms%d" % mc)
                kb.op("dve", lambda e: e.tensor_scalar(memb[mc][:], memx[mc][:], mst[:, 3 * mc + 2:3 * mc + 3], None, ALU.mult),
                      reads=["memx%d" % mc, "mrs%d" % mc], writes=["memb%d" % mc])
                kb.pe([(lambda e, c=c: e.transpose(pTb[:, c * 128:(c + 1) * 128], memb[mc][:, c * 128:(c + 1) * 128], ident_b[:]))
                       for c in range(NCH)], reads=["memb%d" % mc, "ident_b"], writes=["pTb"])
                kb.op("dve", lambda e: e.tensor_copy(memT[:, :, mc * 128:(mc + 1) * 128], pTb[:].rearrange("p (c t) -> p c t", c=NCH)),
                      reads=["pTb"], writes=["memT"])
            for oc in range(8):
                pp = pM[oc % 2]; pk = "pM%d" % (oc % 2)
                kb.pe([(lambda e, c=c: e.matmul(pp[:, 0:MEM], wkv[:, c, oc * 128:(oc + 1) * 128], memT[:, c, :], start=(c == 0), stop=(c == NCH - 1)))
                       for c in range(NCH)], reads=["wkv", "memT"], writes=[pk])
                kb.op("dve", lambda e: e.tensor_copy(kmT[:, oc, :], pp[:, 0:MEM]), reads=[pk], writes=["kmT"])
            for mc in range(2):
                for n in range(2):
                    pp = pOm[n]; pk = "pOm%d" % n
                    kb.pe([(lambda e, c=c: e.matmul(pp[:], memT[:, c, mc * 128:(mc + 1) * 128], wkv[:, c, D + n * 512:D + (n + 1) * 512],
                                                    start=(c == 0), stop=(c == NCH - 1))) for c in range(NCH)],
                          reads=["wkv", "memT"], writes=[pk])
                    kb.op("dve", lambda e: e.tensor_copy(vm[:, mc, n * 512:(n + 1) * 512], pp[:]), reads=[pk], writes=["vm"])
            kb.barrier()

        NB = 2
        act8 = [sb("act8_%d" % i, [128, 8, 512], BF16) for i in range(NB)]
        xs = [[sb("xs%d_%d" % (i, j), [128, D], F32) for j in range(4)] for i in range(NB)]
        junk = sb("junk3", [128, D], BF16)
        st8 = [sb("st83_%d" % i, [128, 8], F32) for i in range(4)]
        st9 = [sb("st93_%d" % i, [128, 8], F32) for i in range(4)]
        h2b = [sb("h2b%d" % i, [128, D], BF16) for i in range(4)]
        h2T = sb("h2T", [128, NCH, 512], BF16)
        qmT = sb("qmT", [128, 8, 512], BF16)
        pm = [[sb("pm%d_%d" % (i, mc), [128, 512], BF16) for mc in range(2)] for i in range(2)]
        Rm = [sb("Rm%d" % i, [128, 512], F32) for i in range(2)]
        om8 = sb("om8", [128, 8, 512], BF16)
        h3f = [sb("h3f%d" % i, [128, D], F32) for i in range(4)]
        h3b = [sb("h3b%d" % i, [128, D], BF16) for i in range(2)]
        h3T = [sb("h3T%d" % i, [128, NCH, 128], F32) for i in range(2)]
        rt = [sb("rt%d" % i, [128, 160], F32) for i in range(4)]
        ohsum = [sb("ohsum%d" % i, [128, NE], BF16) for i in range(4)]
        rtmp = [sb("rtmp%d" % i, [128, 3 * NE], F32) for i in range(2)]

        def rms_stats(xt, xk, st, sk):
            kb.op("act", lambda e: e.activation(junk[:], xt, AF.Square, accum_out=st[:, 0:1]), reads=[xk], writes=["junk3", sk + "ss"])
            kb.op("dve", lambda e: e.tensor_scalar(st[:, 1:2], st[:, 0:1], 1.0 / D, RMS_EPS, ALU.mult, ALU.add), reads=[sk + "ss"], writes=[sk + "ms"])
            rsqrt_col(st[:, 2:3], st[:, 1:2], 1, sk + "rs", sk + "ms")

        def loads(qb):
            m = qb % NB
            kb.dma("sp", lambda e: e.dma_start(out=act8[m][:, 0:4, :], in_=g["aT_s"].ap()[:, :, qb * 512:(qb + 1) * 512].rearrange("h p t -> p h t")),
                   reads=["aT_s"], writes=["act8_%d" % m])
            kb.dma("sp", lambda e: e.dma_start(out=act8[m][:, 4:8, :], in_=g["oT_s"].ap()[:, :, qb * 512:(qb + 1) * 512].rearrange("h p t -> p h t")),
                   reads=["oT_s"], writes=["act8_%d" % m])
            for j in range(4):
                t = qb * 4 + j
                kb.dma("sp", lambda e: e.dma_start(out=xs[m][j][:], in_=x_d[t * 128:(t + 1) * 128, :]), writes=["xs%d_%d" % (m, j)])

        import os
        NQ3 = int(os.environ.get("P3_NQ", NQ))
        router_q = []
        negone = sb("negone", [128, 2], F32)
        kb.op("pool", lambda e: e.memset(negone[:], -1.0), writes=["negone"])

        def router_some(n):
            for _ in range(n):
                if router_q:
                    f_, q_, j_ = router_q.pop(0); f_(q_, j_)

        loads(0)
        for qb in range(NQ3):
            m = qb % NB
            if qb + 1 < NQ3:
                loads(qb + 1)
            def mk_h2b(j):
                xt = xs[m][j]; xk = "xs%d_%d" % (m, j)
                st = st8[j]; sk = "st83_%d" % j
                kb.op("act", lambda e: e.activation(h2b[j][:], xt[:], AF.Copy, scale=st[:, 2:3]), reads=[xk, sk + "rs"], writes=["h2b%d" % j])

            for j in range(4):
                xt = xs[m][j]; xk = "xs%d_%d" % (m, j)
                for n in range(2):
                    kb.pe([(lambda e, c=c: e.matmul(pM[n][:], act8[m][:, c, j * 128:(j + 1) * 128], w_out[:, c, n * 512:(n + 1) * 512],
                                                    start=(c == 0), stop=(c == NCH - 1))) for c in range(NCH)],
                          reads=["act8_%d" % m, "w_out"], writes=["pM%d" % n])
                    kb.op("dve", lambda e: e.tensor_tensor(xt[:, n * 512:(n + 1) * 512], pM[n][:], xt[:, n * 512:(n + 1) * 512], ALU.add),
                          reads=["pM%d" % n, xk], writes=[xk])
                rms_stats(xt[:], xk, st8[j], "st83_%d" % j)
                if j >= 1:
                    mk_h2b(j - 1)
                router_some(2 if j < 2 else 1)
            mk_h2b(3)
            for j in range(4):
                hb_ = h2b[j]; hk = "h2b%d" % j
                kb.pe([(lambda e, c=c: e.transpose(pTb[:, c * 128:(c + 1) * 128], hb_[:, c * 128:(c + 1) * 128], ident_b[:]))
                       for c in range(NCH)], reads=[hk, "ident_b"], writes=["pTb"])
                kb.op("act", lambda e: e.copy(h2T[:, :, j * 128:(j + 1) * 128], pTb[:].rearrange("p (c t) -> p c t", c=NCH)),
                      reads=["pTb"], writes=["h2T_%d" % j])
                router_some(2 if j < 2 else 1)
            router_some(99)
            h2k = ["h2T_%d" % j for j in range(4)]
            def qm(oc):
                pp = pM[oc % 2]; pk = "pM%d" % (oc % 2)
                kb.pe([(lambda e, c=c: e.matmul(pp[:], w_q[:, c, oc * 128:(oc + 1) * 128], h2T[:, c, :], start=(c == 0), stop=(c == NCH - 1)))
                       for c in range(NCH)], reads=["w_q"] + h2k, writes=[pk])
                kb.op("act" if oc % 2 == 0 else "dve",
                      (lambda e: e.activation(qmT[:, oc, :], pp[:], AF.Copy, scale=1.0 / 16)) if oc % 2 == 0 else
                      (lambda e: e.tensor_scalar(qmT[:, oc, :], pp[:], 1.0 / 16, None, ALU.mult)),
                      reads=[pk], writes=["qmT_%d" % oc])

            qm(0); qm(1)
            for h in range(4):
                pb = h % 2
                for mc in range(2):
                    kb.pe([(lambda e, c2=c2: e.matmul(pSc[:, mc * 512:(mc + 1) * 512], kmT[:, h * 2 + c2, mc * 128:(mc + 1) * 128], qmT[:, h * 2 + c2, :],
                                                      start=(c2 == 0), stop=(c2 == 1))) for c2 in range(2)],
                          reads=["kmT", "qmT_%d" % (2 * h), "qmT_%d" % (2 * h + 1)], writes=["pSc%d" % mc])
                    kb.op("act", lambda e: e.activation(pm[pb][mc][:], pSc[:, mc * 512:(mc + 1) * 512], AF.Exp),
                          reads=["pSc%d" % mc], writes=["pm%d_%d" % (pb, mc)])
                if h + 1 < 4:
                    qm(2 * h + 2); qm(2 * h + 3)
                pmk = ["pm%d_%d" % (pb, mc) for mc in range(2)]
                kb.pe([(lambda e, mc=mc: e.matmul(pLm[:], ones_b[:], pm[pb][mc][:], start=(mc == 0), stop=(mc == 1))) for mc in range(2)],
                      reads=["ones_b"] + pmk, writes=["pLm", "pLm"])
                kb.op("dve", lambda e: e.reciprocal(Rm[pb][:], pLm[:]), reads=["pLm"], writes=["Rm%d" % pb])
                for c2 in range(2):
                    kb.pe([(lambda e, mc=mc: e.matmul(pOm[c2][:], vm[:, mc, h * 256 + c2 * 128:h * 256 + (c2 + 1) * 128], pm[pb][mc][:],
                                                      start=(mc == 0), stop=(mc == 1))) for mc in range(2)],
                          reads=["vm"] + pmk, writes=["pOm%d" % c2])
                    kb.op("dve", lambda e: e.tensor_tensor(om8[:, h * 2 + c2, :], pOm[c2][:], Rm[pb][:], ALU.mult),
                          reads=["pOm%d" % c2, "Rm%d" % pb], writes=["om8_%d" % (h * 2 + c2)])
            omk = ["om8_%d" % i for i in range(8)]
            for j in range(4):
                t = qb * 4 + j
                xt = xs[m][j]; xk = "xs%d_%d" % (m, j)
                for n in range(2):
                    kb.pe([(lambda e, c=c: e.matmul(pM[n][:], om8[:, c, j * 128:(j + 1) * 128], w_o[:, c, n * 512:(n + 1) * 512],
                                                    start=(c == 0), stop=(c == NCH - 1))) for c in range(NCH)],
                          reads=omk + ["w_o"], writes=["pM%d" % n])
                    kb.op("dve", lambda e: e.tensor_tensor(xt[:, n * 512:(n + 1) * 512], pM[n][:], xt[:, n * 512:(n + 1) * 512], ALU.add),
                          reads=["pM%d" % n, xk], writes=[xk])
                kb.dma("sp", lambda e: e.dma_start(out=g["x2_s"][t * 128:(t + 1) * 128, :], in_=xt[:]), reads=[xk], writes=["x2_s"])
                st = st9[j]; sk = "st93_%d" % j
                rms_stats(xt[:], xk, st, sk)
                hf = h3f[j]; hfk = "h3f%d" % j
                kb.op("dve", lambda e: e.scalar_tensor_tensor(hf[:], xt[:], st[:, 2:3], gffn[:], ALU.mult, ALU.mult),
                      reads=[xk, sk + "rs", "gffn"], writes=[hfk])
                kb.op("act", lambda e: e.copy(h3b[j % 2][:], hf[:]), reads=[hfk], writes=["h3b%d" % (j % 2)])
                kb.dma("sp", lambda e: e.dma_start(out=g["h3_s"][t * 128:(t + 1) * 128, :], in_=h3b[j % 2][:]), reads=["h3b%d" % (j % 2)], writes=["h3_s"])

            def r_T(qb, j):
                hf = h3f[j]; hfk = "h3f%d" % j
                r = j % 2
                pbank = pSc if r == 0 else None
                if r == 0:
                    kb.pe([(lambda e, c=c: e.transpose(pSc[:, c * 128:(c + 1) * 128], hf[:, c * 128:(c + 1) * 128], ident_f[:]))
                           for c in range(NCH)], reads=[hfk, "ident_f"], writes=["pSc0", "pSc1"])
                    kb.op("dve", lambda e: e.tensor_copy(h3T[r][:, 0:4, :], pSc[:, 0:512].rearrange("p (c t) -> p c t", c=4)),
                          reads=["pSc0"], writes=["h3T%d" % r])
                    kb.op("act", lambda e: e.copy(h3T[r][:, 4:8, :], pSc[:, 512:1024].rearrange("p (c t) -> p c t", c=4)),
                          reads=["pSc1"], writes=["h3T%d" % r])
                else:
                    kb.pe([(lambda e, c=c: e.transpose(pOm[c // 4][:, (c % 4) * 128:(c % 4 + 1) * 128], hf[:, c * 128:(c + 1) * 128], ident_f[:]))
                           for c in range(NCH)], reads=[hfk, "ident_f"], writes=["pOm0", "pOm1"])
                    kb.op("dve", lambda e: e.tensor_copy(h3T[r][:, 0:4, :], pOm[0][:].rearrange("p (c t) -> p c t", c=4)),
                          reads=["pOm0"], writes=["h3T%d" % r])
                    kb.op("act", lambda e: e.copy(h3T[r][:, 4:8, :], pOm[1][:].rearrange("p (c t) -> p c t", c=4)),
                          reads=["pOm1"], writes=["h3T%d" % r])

            def r_L(qb, j):
                t = qb * 4 + j
                r = j % 2
                kb.pe([(lambda e, c=c: e.matmul(pLm[:, 0:36], h3T[r][:, c, :], w_r[:, c, :], start=(c == 0), stop=(c == NCH - 1)))
                       for c in range(NCH)], reads=["h3T%d" % r, "w_r"], writes=["pLm"])
                R_ = rt[j]
                rk_ = "rt%d_" % j
                L = R_[:, 0:36]
                kb.op("dve", lambda e: e.tensor_tensor(L, pLm[:, 0:36], b_r[:], ALU.add), reads=["pLm", "b_r"], writes=[rk_ + "L"])
                kb.op("dve", lambda e: e.reduce_max(R_[:, 36:37], R_[:, 0:4], AX.X), reads=[rk_ + "L"], writes=[rk_ + "gmax"])
                kb.op("dve", lambda e: e.tensor_scalar(R_[:, 40:44], R_[:, 0:4], R_[:, 36:37], None, ALU.is_equal),
                      reads=[rk_ + "L", rk_ + "gmax"], writes=[rk_ + "ohg"])
                kb.op("dve", lambda e: e.tensor_scalar(R_[:, 37:38], R_[:, 36:37], -1.0, None, ALU.mult), reads=[rk_ + "gmax"], writes=[rk_ + "ngmax"])
                kb.op("act", lambda e: e.activation(R_[:, 44:48], R_[:, 0:4], AF.Exp, bias=R_[:, 37:38], scale=1.0, accum_out=R_[:, 38:39]),
                      reads=[rk_ + "L", rk_ + "ngmax"], writes=[rk_ + "eg", rk_ + "gsum"])
                kb.op("dve", lambda e: e.reciprocal(R_[:, 39:40], R_[:, 38:39]), reads=[rk_ + "gsum"], writes=[rk_ + "gate"])
                kb.op("dve", lambda e: e.tensor_scalar(R_[:, 48:52], R_[:, 40:44], -1.0, 1e30, ALU.add, ALU.mult), reads=[rk_ + "ohg"], writes=[rk_ + "pen"])
                for gi in range(4):
                    kb.op("dve", lambda e, gi=gi: e.tensor_scalar(R_[:, 56 + gi * 8:64 + gi * 8], R_[:, 4 + gi * 8:12 + gi * 8],
                                                                  R_[:, 48 + gi:49 + gi], None, ALU.add),
                          reads=[rk_ + "L", rk_ + "pen"], writes=[rk_ + "elm%d" % gi])
                elk = [rk_ + "elm%d" % gi for gi in range(4)]
                elm = R_[:, 56:88]
                kb.op("dve", lambda e: e.reduce_max(R_[:, 52:53], elm, AX.X), reads=elk, writes=[rk_ + "m1"])
                oh1 = OHs[:, t, 0, :]; oh2 = OHs[:, t, 1, :]
                kb.op("dve", lambda e: e.tensor_scalar(oh1, elm, R_[:, 52:53], None, ALU.is_equal), reads=elk + [rk_ + "m1"], writes=["OHs"])
                elm2 = R_[:, 88:120]
                kb.op("dve", lambda e: e.tensor_scalar(elm2, oh1, -1e30, None, ALU.mult), reads=["OHs"], writes=[rk_ + "elm2"])
                kb.op("dve", lambda e: e.tensor_tensor(elm2, elm2, elm, ALU.add), reads=elk + [rk_ + "elm2"], writes=[rk_ + "elm2"])
                kb.op("dve", lambda e: e.reduce_max(R_[:, 53:54], elm2, AX.X), reads=[rk_ + "elm2"], writes=[rk_ + "m2"])
                kb.op("dve", lambda e: e.tensor_scalar(oh2, elm2, R_[:, 53:54], None, ALU.is_equal), reads=[rk_ + "elm2", rk_ + "m2"], writes=["OHs"])
                kb.op("dve", lambda e: e.tensor_tensor(R_[:, 54:55], R_[:, 53:54], R_[:, 52:53], ALU.subtract),
                      reads=[rk_ + "m1", rk_ + "m2"], writes=[rk_ + "d"])
                kb.op("act", lambda e: e.activation(R_[:, 55:56], R_[:, 54:55], AF.Exp), reads=[rk_ + "d"], writes=[rk_ + "ed"])
                kb.op("dve", lambda e: e.tensor_scalar(R_[:, 120:121], R_[:, 55:56], 1.0, None, ALU.add), reads=[rk_ + "ed"], writes=[rk_ + "den"])
                kb.op("dve", lambda e: e.reciprocal(R_[:, 121:122], R_[:, 120:121]), reads=[rk_ + "den"], writes=[rk_ + "rden"])
                kb.op("dve", lambda e: e.tensor_tensor(WT[:, t, 0:1], R_[:, 121:122], R_[:, 39:40], ALU.mult),
                      reads=[rk_ + "rden", rk_ + "gate"], writes=["WT"])
                kb.op("dve", lambda e: e.tensor_tensor(WT[:, t, 1:2], R_[:, 39:40], WT[:, t, 0:1], ALU.subtract),
                      reads=[rk_ + "gate", "WT"], writes=["WT"])
                kb.op("dve", lambda e: e.tensor_tensor(ohsum[j][:], oh1, oh2, ALU.add), reads=["OHs"], writes=["ohsum%d" % j])

            def r_P(qb, j):
                t = qb * 4 + j
                r = j % 2
                kb.pe([lambda e: e.matmul(pLm[:, 64:64 + NE], tri_b[:], ohsum[j][:], start=True, stop=True),
                       lambda e: e.matmul(pLm[:, 64 + NE:64 + 2 * NE], ones_b[:], ohsum[j][:], start=True, stop=True)],
                      reads=["tri_b", "ones_b", "ohsum%d" % j], writes=["pLm"])
                T_ = rtmp[r]; tk = "rtmp%d_" % r
                kb.op("dve", lambda e: e.tensor_tensor(T_[:, 0:NE], pLm[:, 64:64 + NE], BASE[:], ALU.add), reads=["pLm", "BASE"], writes=[tk + "a"])
                for k in range(2):
                    kb.op("dve", lambda e, k=k: e.tensor_tensor(T_[:, (1 + k) * NE:(2 + k) * NE], OHs[:, t, k, :], T_[:, 0:NE], ALU.mult),
                          reads=["OHs", tk + "a"], writes=[tk + "b%d" % k])
                    kb.op("dve", lambda e, k=k: e.reduce_sum(RK[:, t, k:k + 1], T_[:, (1 + k) * NE:(2 + k) * NE], AX.X),
                          reads=[tk + "b%d" % k], writes=["RK"])
                kb.op("dve", lambda e: e.tensor_tensor(BASE[:], pLm[:, 64 + NE:64 + 2 * NE], BASE[:], ALU.add), reads=["pLm", "BASE"], writes=["BASE"])

            router_q.extend([(r_T, qb, 0), (r_T, qb, 1), (r_L, qb, 0), (r_T, qb, 2), (r_L, qb, 1), (r_T, qb, 3),
                             (r_L, qb, 2), (r_P, qb, 0), (r_L, qb, 3), (r_P, qb, 1), (r_P, qb, 2), (r_P, qb, 3)])
        while router_q:
            f_, q_, j_ = router_q.pop(0); f_(q_, j_)
        kb.barrier()


def phase4(nc, kb, g):
    S = g["S"]; NT = g["NT"]; NBLK = g["NBLK"]; P_ROWS = g["P_ROWS"]
    ident_b = g["ident_b"]; rsqrt_col = g["rsqrt_col"]
    OHs = g["OHs"]; RK = g["RK"]; WT = g["WT"]; BASE = g["BASE"]
    with ExitStack() as es:
        sb = lambda name, shape, dtype: es.enter_context(nc.sbuf_tensor("s_" + name, list(shape), dtype))
        ps = lambda name, shape, dtype: es.enter_context(nc.psum_tensor("p_" + name, list(shape), dtype))
        posi = sb("posi", [128, NT * 2], I32)
        idxw = sb("idxw", [128, NBLK], I32)
        gfin = sb("gfin", [128, D], F32)
        es_t = ExitStack()
        sbt = lambda name, shape, dtype: es_t.enter_context(nc.sbuf_tensor("s_" + name, list(shape), dtype))
        ci = sbt("ci", [128, NE], I32)
        cf = [sbt("cf%d" % i, [128, NE], F32) for i in range(4)]
        thr_i = sbt("thr_i", [128, NBLK], I32)
        thr = sbt("thr", [128, NBLK], F32)
        cmp_ = sbt("cmp", [128, NBLK, NE], F32)
        bef = sbt("bef", [128, 2 * NBLK], F32)
        ptmp = sbt("ptmp", [128, NT * 2, NE], F32)
        posf = sbt("posf", [128, NT * 2], F32)
        pidx_i = sbt("pidx_i", [128, 1], I32)
        pidx = sbt("pidx", [128, 1], F32)
        idxw_f = sbt("idxw_f", [128, NBLK], F32)
        hrow = [sbt("hrow%d" % i, [128, D], BF16) for i in range(6)]
        kb.dma("sp", lambda e: e.dma_start(out=gfin[:], in_=bcast_rows(g["norm_final_d"].ap(), 128, D)), writes=["gfin"])

        kb.op("dve", lambda e: e.tensor_scalar(cf[0][:], BASE[:], float(RB - 1), None, ALU.add), reads=["BASE"], writes=["cf0"])
        kb.op("dve", lambda e: e.tensor_copy(ci[:], cf[0][:]), reads=["cf0"], writes=["ci"])
        kb.op("dve", lambda e: e.tensor_single_scalar(ci[:], ci[:], 8, ALU.arith_shift_right), reads=["ci"], writes=["ci"])
        kb.op("dve", lambda e: e.tensor_single_scalar(ci[:], ci[:], 8, ALU.logical_shift_left), reads=["ci"], writes=["ci"])
        kb.op("dve", lambda e: e.tensor_copy(cf[1][:], ci[:]), reads=["ci"], writes=["cf1"])
        kb.op("dve", lambda e: e.tensor_copy(cf[2][:], cf[1][:]), reads=["cf1"], writes=["cf2"])
        cur, nxt = 2, 3
        for sh in (1, 2, 4, 8, 16):
            kb.op("dve", lambda e: e.tensor_copy(cf[nxt][:, 0:sh], cf[cur][:, 0:sh]), reads=["cf%d" % cur], writes=["cf%d" % nxt])
            kb.op("dve", lambda e: e.tensor_tensor(cf[nxt][:, sh:NE], cf[cur][:, sh:NE], cf[cur][:, 0:NE - sh], ALU.add),
                  reads=["cf%d" % cur], writes=["cf%d" % nxt])
            cur, nxt = nxt, cur
        pend = cf[cur]; pendk = "cf%d" % cur
        pst = cf[nxt]; pstk = "cf%d" % nxt
        kb.op("dve", lambda e: e.tensor_tensor(pst[:], pend[:], cf[1][:], ALU.subtract), reads=[pendk, "cf1"], writes=[pstk])
        kb.op("pool", lambda e: e.iota(thr_i[:], [[RB, NBLK]], base=0, channel_multiplier=0), writes=["thr_i"])
        kb.op("dve", lambda e: e.tensor_copy(thr[:], thr_i[:]), reads=["thr_i"], writes=["thr"])
        kb.op("dve", lambda e: e.tensor_tensor(cmp_[:], pend[:].unsqueeze(1).broadcast_to([128, NBLK, NE]),
                                               thr[:].unsqueeze(2).broadcast_to([128, NBLK, NE]), ALU.is_le),
              reads=[pendk, "thr"], writes=["cmp"])
        kb.op("dve", lambda e: e.reduce_sum(bef[:, 0:NBLK], cmp_[:], AX.X), reads=["cmp"], writes=["bef0"])
        kb.op("dve", lambda e: e.tensor_scalar(bef[:, 0:NBLK], bef[:, 0:NBLK], float(NE - 1), None, ALU.min),
              reads=["bef0"], writes=["bef0"])
        kb.op("dve", lambda e: e.tensor_tensor(ptmp[:], OHs[:].rearrange("p t k e -> p (t k) e"),
                                               pst[:].unsqueeze(1).broadcast_to([128, NT * 2, NE]), ALU.mult),
              reads=["OHs", pstk], writes=["ptmp"])
        kb.op("dve", lambda e: e.reduce_sum(posf[:], ptmp[:], AX.X), reads=["ptmp"], writes=["posf"])
        kb.op("dve", lambda e: e.scalar_tensor_tensor(posf[:], posf[:], -1.0, RK[:].rearrange("p t k -> p (t k)"), ALU.add, ALU.add),
              reads=["posf", "RK"], writes=["posf"])
        kb.op("dve", lambda e: e.tensor_copy(posi[:], posf[:]), reads=["posf"], writes=["posi"])

        kb.op("pool", lambda e: e.iota(pidx_i[:], [[0, 1]], base=0, channel_multiplier=1), writes=["pidx_i"])
        kb.op("dve", lambda e: e.tensor_copy(pidx[:], pidx_i[:]), reads=["pidx_i"], writes=["pidx"])
        kb.op("dve", lambda e: e.tensor_scalar(idxw_f[:], bef[:, 0:NBLK], 128.0, pidx[:, 0:1], ALU.mult, ALU.add),
              reads=["bef0", "pidx"], writes=["idxw_f"])
        kb.op("dve", lambda e: e.tensor_copy(idxw[:], idxw_f[:]), reads=["idxw_f"], writes=["idxw"])
        bc_rows = nc.gpsimd.to_reg(P_ROWS - 1)
        bc_w = nc.gpsimd.to_reg(NE * 128 - 1)
        for t in range(NT):
            hb_ = hrow[t % 6]; hk = "hrow%d" % (t % 6)
            kb.dma("sp", lambda e: e.dma_start(out=hb_[:], in_=g["h3_s"][t * 128:(t + 1) * 128, :]), reads=["h3_s"], writes=[hk])
            for k in range(2):
                kb.dma("pool", lambda e: e.indirect_dma_start(
                    out=g["xrows_s"][:, :], out_offset=bass.IndirectOffsetOnAxis(ap=posi[:, t * 2 + k:t * 2 + k + 1], axis=0),
                    in_=hb_[:], in_offset=None, bounds_check=bc_rows, oob_is_err=False),
                    reads=[hk, "posi"], writes=["xrows_s"])

        wall_s = g["wall_s"]
        kb.barrier()
        es_t.close()
        NWB = 3
        wbuf = [sb("wbuf%d" % i, [128, 12288], BF16) for i in range(NWB)]

        def load_weights(b):
            wb = b % NWB
            kb.dma("pool", lambda e: e.indirect_dma_start(
                out=wbuf[wb][:], out_offset=None, in_=wall_s[:, :],
                in_offset=bass.IndirectOffsetOnAxis(ap=idxw[:, b:b + 1], axis=0),
                bounds_check=bc_w, oob_is_err=False),
                reads=["wall_s", "idxw"], writes=["wbuf%d" % wb])

        kb.barrier()
        NXR = 6
        xr = [sb("xr%d" % i, [128, D], BF16) for i in range(NXR)]
        xT = [sb("xT%d" % i, [128, NCH, 128], BF16) for i in range(3)]
        sg = [sb("sg%d" % i, [128, DE], F32) for i in range(2)]
        ab = [sb("ab%d" % i, [128, DE], BF16) for i in range(2)]
        aT = [sb("aT%d" % i, [128, 4, 128], BF16) for i in range(2)]
        ysb = [sb("ysb%d" % i, [128, D], F32) for i in range(3)]
        pXT = [ps("p4XT%d" % i, [128, 1024], BF16) for i in range(2)]
        pG = ps("p4G", [128, 512], F32)
        pU = ps("p4U", [128, 512], F32)
        pAT = ps("p4AT", [128, 1024], BF16)
        pY = [ps("p4Y%d" % i, [128, 512], F32) for i in range(2)]
        NH = 2 * NBLK

        def st_load(i):
            r = i % NXR
            row0 = i * 128
            kb.dma("act", lambda e: e.dma_start(out=xr[r][:], in_=g["xrows_s"][row0:row0 + 128, :]), reads=["xrows_s"], writes=["xr%d" % r])

        def st_T(i):
            r = i % NXR; p = i % 2; x3 = i % 3
            kb.pe([(lambda e, c=c: e.transpose(pXT[p][:, c * 128:(c + 1) * 128], xr[r][:, c * 128:(c + 1) * 128], ident_b[:]))
                   for c in range(NCH)], reads=["xr%d" % r, "ident_b"], writes=["pXT%d" % p])
            if i % 2 == 0:
                kb.op("dve", lambda e: e.tensor_copy(xT[x3][:], pXT[p][:].rearrange("p (c t) -> p c t", c=NCH)), reads=["pXT%d" % p], writes=["xT%d" % x3])
            else:
                kb.op("act", lambda e: e.copy(xT[x3][:], pXT[p][:].rearrange("p (c t) -> p c t", c=NCH)), reads=["pXT%d" % p], writes=["xT%d" % x3])

        def st_GU(i):
            x3 = i % 3; r2 = i % 2; wb = (i // 2) % NWB
            emits = []
            for c in range(NCH):
                emits.append(lambda e, c=c: e.matmul(pG[:], xT[x3][:, c, :], wbuf[wb][:, c * 512:(c + 1) * 512], start=(c == 0), stop=(c == NCH - 1)))
                emits.append(lambda e, c=c: e.matmul(pU[:], xT[x3][:, c, :], wbuf[wb][:, 4096 + c * 512:4096 + (c + 1) * 512], start=(c == 0), stop=(c == NCH - 1)))
            kb.pe(emits, reads=["xT%d" % x3, "wbuf%d" % wb], writes=["pG", "pU"])
            kb.op("act", lambda e: e.activation(sg[r2][:], pG[:], AF.Silu), reads=["pG"], writes=["sg%d" % r2])
            kb.op("dve", lambda e: e.tensor_tensor(ab[r2][:], pU[:], sg[r2][:], ALU.mult), reads=["pU", "sg%d" % r2], writes=["ab%d" % r2])

        def st_TA(i):
            r2 = i % 2
            kb.pe([(lambda e, c=c: e.transpose(pAT[:, c * 128:(c + 1) * 128], ab[r2][:, c * 128:(c + 1) * 128], ident_b[:]))
                   for c in range(4)], reads=["ab%d" % r2, "ident_b"], writes=["pAT"])
            kb.op("dve", lambda e: e.tensor_copy(aT[r2][:], pAT[:, 0:512].rearrange("p (c t) -> p c t", c=4)), reads=["pAT"], writes=["aT%d" % r2])

        def st_DOWN(i):
            r2 = i % 2; y3 = i % 3; wb = (i // 2) % NWB
            row0 = i * 128
            for n in range(2):
                kb.pe([(lambda e, c=c: e.matmul(pY[n][:], aT[r2][:, c, :], wbuf[wb][:, 8192 + c * 1024 + n * 512:8192 + c * 1024 + (n + 1) * 512],
                                                start=(c == 0), stop=(c == 3))) for c in range(4)],
                      reads=["aT%d" % r2, "wbuf%d" % wb], writes=["pY%d" % n])
            kb.op("act", lambda e: e.copy(ysb[y3][:, 0:512], pY[0][:]), reads=["pY0"], writes=["ysb%d_0" % y3])
            kb.op("dve", lambda e: e.tensor_copy(ysb[y3][:, 512:1024], pY[1][:]), reads=["pY1"], writes=["ysb%d_1" % y3])
            kb.dma("sp", lambda e: e.dma_start(out=g["yrows_s"][row0:row0 + 128, :], in_=ysb[y3][:]),
                   reads=["ysb%d_0" % y3, "ysb%d_1" % y3], writes=["yrows_s"])

        for b_ in range(min(NWB, NBLK)):
            load_weights(b_)
        for i in range(min(5, NH)):
            st_load(i)
        st_T(0); st_T(1)
        st_GU(0)
        for s_ in range(NH):
            if s_ + 5 < NH:
                st_load(s_ + 5)
            if s_ + 2 < NH:
                st_T(s_ + 2)
            st_TA(s_)
            if s_ + 1 < NH:
                st_GU(s_ + 1)
            st_DOWN(s_)
            if s_ % 2 == 1 and (s_ // 2) + NWB < NBLK:
                load_weights(s_ // 2 + NWB)

        kb.barrier()
        NR = 3
        y1 = [sb("y1_%d" % i, [128, D], F32) for i in range(NR)]
        y2 = [sb("y2_%d" % i, [128, D], F32) for i in range(NR)]
        x2t = [sb("x2t%d" % i, [128, D], F32) for i in range(NR)]
        fjunk = sb("fjunk", [128, D], BF16)
        fst = [sb("fst%d" % i, [128, 8], F32) for i in range(NR)]
        fo = [sb("fo%d" % i, [128, D], F32) for i in range(NR)]

        def cb_load(t):
            r = t % NR
            for k, yy in ((0, y1), (1, y2)):
                kb.dma("pool", lambda e: e.indirect_dma_start(
                    out=yy[r][:], out_offset=None, in_=g["yrows_s"][:, :],
                    in_offset=bass.IndirectOffsetOnAxis(ap=posi[:, t * 2 + k:t * 2 + k + 1], axis=0),
                    bounds_check=bc_rows, oob_is_err=False),
                    reads=["yrows_s", "posi"], writes=["y%d_%d" % (k + 1, r)])
            kb.dma("sp", lambda e: e.dma_start(out=x2t[r][:], in_=g["x2_s"][t * 128:(t + 1) * 128, :]), reads=["x2_s"], writes=["x2t%d" % r])

        for t in range(min(2, NT)):
            cb_load(t)
        for t in range(NT):
            r = t % NR
            if t + 2 < NT:
                cb_load(t + 2)
            kb.op("dve", lambda e: e.scalar_tensor_tensor(x2t[r][:], y1[r][:], WT[:, t, 0:1], x2t[r][:], ALU.mult, ALU.add),
                  reads=["y1_%d" % r, "WT", "x2t%d" % r], writes=["x2t%d" % r])
            kb.op("dve", lambda e: e.scalar_tensor_tensor(x2t[r][:], y2[r][:], WT[:, t, 1:2], x2t[r][:], ALU.mult, ALU.add),
                  reads=["y2_%d" % r, "WT", "x2t%d" % r], writes=["x2t%d" % r])
            st = fst[r]; sk = "fst%d" % r
            kb.op("act", lambda e: e.activation(fjunk[:], x2t[r][:], AF.Square, accum_out=st[:, 0:1]), reads=["x2t%d" % r], writes=["fjunk", sk + "ss"])
            kb.op("dve", lambda e: e.tensor_scalar(st[:, 1:2], st[:, 0:1], 1.0 / D, RMS_EPS, ALU.mult, ALU.add), reads=[sk + "ss"], writes=[sk + "ms"])
            rsqrt_col(st[:, 2:3], st[:, 1:2], 1, sk + "rs", sk + "ms")
            kb.op("dve", lambda e: e.scalar_tensor_tensor(fo[r][:], x2t[r][:], st[:, 2:3], gfin[:], ALU.mult, ALU.mult),
                  reads=["x2t%d" % r, sk + "rs", "gfin"], writes=["fo%d" % r])
            kb.dma("sp", lambda e: e.dma_start(out=g["out_d"][t * 128:(t + 1) * 128, :], in_=fo[r][:]), reads=["fo%d" % r], writes=["out"])
        kb.barrier()


def core_inputs(inp, b):
    f = lambda a: np.ascontiguousarray(np.asarray(a, dtype=np.float32))
    return {
        "x": f(inp["x"][b]),
        "mem": f(inp["mem"][b]),
        "norm_mix": f(inp["norm_mix"]).reshape(1, D),
        "w_in": f(inp["w_in"][0]),
        "sgu_ln_g": f(inp["sgu_ln_g"][0]),
        "sgu_ln_b": f(inp["sgu_ln_b"][0]),
        "sgu_w": f(inp["sgu_w"][0]),
        "sgu_b": f(inp["sgu_b"][0]).reshape(1, 512),
        "lam4": f(np.concatenate([np.asarray(inp[k]).reshape(-1) for k in
                                  ("lambda_q1", "lambda_k1", "lambda_q2", "lambda_k2")])).reshape(1, 256),
        "diff_subln": f(inp["diff_subln"]).reshape(1, 128),
        "w_out": f(inp["w_out"][0]),
        "norm_xq": f(inp["norm_xq"]).reshape(1, D),
        "norm_mem": f(inp["norm_mem"]).reshape(1, D),
        "w_q_mem": f(inp["w_q_mem"][0]),
        "w_kv_mem": f(inp["w_kv_mem"][0]),
        "w_o_mem": f(inp["w_o_mem"][0]),
        "norm_ffn": f(inp["norm_ffn"]).reshape(1, D),
        "w_router": f(np.concatenate([np.asarray(inp["w_router_group"][0]), np.asarray(inp["w_router_expert"][0])], axis=1)),
        "b_router": f(np.concatenate([np.asarray(inp["b_router_group"]).reshape(-1),
                                      np.asarray(inp["b_router_expert"]).reshape(-1)])).reshape(1, 36),
        "w_gate": f(inp["w_gate"][0]).reshape(NE * D, DE),
        "w_up": f(inp["w_up"][0]).reshape(NE * D, DE),
        "w_down": f(inp["w_down"][0]).reshape(NE * DE, D),
        "norm_final": f(inp["norm_final"]).reshape(1, D),
    }


_PROGRAM_CACHE = {}


def kernel(**inputs):
    B, S, _ = np.asarray(inputs["x"]).shape
    if S not in _PROGRAM_CACHE:
        _PROGRAM_CACHE[S] = build_program(S)
    nc = _PROGRAM_CACHE[S]
    in_maps = [core_inputs(inputs, b) for b in range(B)]
    res = run_bass_kernel_spmd(nc, in_maps, core_ids=list(range(B)))
    out = np.stack([np.asarray(res.results[b]["out"], dtype=np.float32).reshape(S, D) for b in range(B)], axis=0)
    return out
```
